# Optimizing a Trainium2 kernel written in Bass

```python
import jax, jax.numpy as jnp
from jax import lax
import numpy as np

D_MODEL = 1024
BATCH = 4
SEQ = 4096
DEPTH = 4

N_MIXERS = 3
N_FOX = (DEPTH + 2) // 3
N_MOBA = (DEPTH + 1) // 3
N_RET = DEPTH // 3

FOX_HEADS = 8
FOX_HD = D_MODEL // FOX_HEADS
FOX_QBLOCK = 128
FOX_IN = 3 * D_MODEL + FOX_HEADS

MOBA_HEADS = 8
MOBA_HD = D_MODEL // MOBA_HEADS
MOBA_BLOCK = 256
MOBA_TOPK = 3
MOBA_QCHUNK = 16
MOBA_IN = 3 * D_MODEL

RET_HEADS = 4
RET_DK = D_MODEL // RET_HEADS
RET_DV = 2 * RET_DK
RET_QK = RET_HEADS * RET_DK
RET_V = RET_HEADS * RET_DV
RET_IN = 2 * RET_QK + 2 * RET_V
RET_CHUNK = 128
RET_ROPE_BASE = 10000.0
RET_GN_EPS = 1e-6

N_EXPERTS = 32
TOP_K = 4
D_FF = D_MODEL
SWIGLU_LIMIT = 7.0
SWIGLU_ALPHA = 1.702
MOE_BLOCK = 256

LN_EPS = 1e-5
DEEPNORM_ALPHA = (2 * DEPTH) ** 0.25
DEEPNORM_BETA = (8 * DEPTH) ** -0.25

kernel_name = 'fox_moba_retnet_moe_deepnorm_trunk'


def layer_norm(x, g, b):
    xf = x.astype(jnp.float32)
    mu = jnp.mean(xf, axis=-1, keepdims=True)
    var = jnp.mean(jnp.square(xf - mu), axis=-1, keepdims=True)
    return ((xf - mu) * lax.rsqrt(var + LN_EPS) * g + b).astype(x.dtype)


def _heads(t, n_heads, hd):
    B, S, _ = t.shape
    return t.reshape(B, S, n_heads, hd).transpose(0, 2, 1, 3)


def forgetting_attention(x, w_in, b_f, w_out):
    B, S, D = x.shape
    proj = x @ w_in
    q = _heads(proj[..., :D], FOX_HEADS, FOX_HD)
    k = _heads(proj[..., D:2 * D], FOX_HEADS, FOX_HD)
    v = _heads(proj[..., 2 * D:3 * D], FOX_HEADS, FOX_HD)
    log_f = jax.nn.log_sigmoid((proj[..., 3 * D:] + b_f).astype(jnp.float32))
    c = jnp.cumsum(log_f, axis=1).transpose(0, 2, 1)
    nq = S // FOX_QBLOCK
    q_blocks = q.reshape(B, FOX_HEADS, nq, FOX_QBLOCK, FOX_HD).transpose(2, 0, 1, 3, 4)
    c_blocks = c.reshape(B, FOX_HEADS, nq, FOX_QBLOCK).transpose(2, 0, 1, 3)
    k_pos = jnp.arange(S)
    scale = FOX_HD ** -0.5

    def block(args):
        qi, ci, i = args
        s = jnp.einsum('bhqd,bhkd->bhqk', qi, k).astype(jnp.float32) * scale
        s = s + ci[..., None] - c[:, :, None, :]
        q_pos = i * FOX_QBLOCK + jnp.arange(FOX_QBLOCK)
        s = jnp.where(k_pos[None, :] <= q_pos[:, None], s, -jnp.inf)
        p = jax.nn.softmax(s, axis=-1).astype(v.dtype)
        return jnp.einsum('bhqk,bhkd->bhqd', p, v)

    o = lax.map(block, (q_blocks, c_blocks, jnp.arange(nq)))
    o = o.transpose(1, 0, 3, 2, 4).reshape(B, S, D)
    return o @ w_out


def moba_attention(x, w_in, w_out):
    B, S, D = x.shape
    proj = x @ w_in
    q = _heads(proj[..., :D], MOBA_HEADS, MOBA_HD)
    k = _heads(proj[..., D:2 * D], MOBA_HEADS, MOBA_HD)
    v = _heads(proj[..., 2 * D:], MOBA_HEADS, MOBA_HD)
    nb = -(-S // MOBA_BLOCK)
    pad = nb * MOBA_BLOCK - S
    k_blk = jnp.pad(k, ((0, 0), (0, 0), (0, pad), (0, 0))).reshape(B, MOBA_HEADS, nb, MOBA_BLOCK, MOBA_HD)
    v_blk = jnp.pad(v, ((0, 0), (0, 0), (0, pad), (0, 0))).reshape(B, MOBA_HEADS, nb, MOBA_BLOCK, MOBA_HD)
    k_mean = jnp.mean(k_blk.astype(jnp.float32), axis=3)
    topk = min(MOBA_TOPK, nb)
    nqc = S // MOBA_QCHUNK
    q_chunks = q.reshape(B, MOBA_HEADS, nqc, MOBA_QCHUNK, MOBA_HD).transpose(2, 0, 1, 3, 4)
    b_ix = jnp.arange(B)[:, None, None, None]
    h_ix = jnp.arange(MOBA_HEADS)[None, :, None, None]
    blk_ids = jnp.arange(nb)
    scale = MOBA_HD ** -0.5

    def chunk(args):
        qi, i = args
        q_pos = i * MOBA_QCHUNK + jnp.arange(MOBA_QCHUNK)
        own = (i * MOBA_QCHUNK) // MOBA_BLOCK
        gate = jnp.einsum('bhqd,bhnd->bhqn', qi.astype(jnp.float32), k_mean)
        gate = jnp.where(blk_ids < own, gate, -jnp.inf)
        _, sel = lax.top_k(gate, topk)
        valid = jnp.arange(topk) < own
        k_sel = k_blk[b_ix, h_ix, sel]
        v_sel = v_blk[b_ix, h_ix, sel]
        s_sel = jnp.einsum('bhqd,bhqnkd->bhqnk', qi, k_sel).astype(jnp.float32) * scale
        s_sel = jnp.where(valid[:, None], s_sel, -jnp.inf).reshape(B, MOBA_HEADS, MOBA_QCHUNK, topk * MOBA_BLOCK)
        k_own = lax.dynamic_index_in_dim(k_blk, own, axis=2, keepdims=False)
        v_own = lax.dynamic_index_in_dim(v_blk, own, axis=2, keepdims=False)
        s_own = jnp.einsum('bhqd,bhkd->bhqk', qi, k_own).astype(jnp.float32) * scale
        own_pos = own * MOBA_BLOCK + jnp.arange(MOBA_BLOCK)
        s_own = jnp.where(own_pos[None, :] <= q_pos[:, None], s_own, -jnp.inf)
        p = jax.nn.softmax(jnp.concatenate([s_sel, s_own], axis=-1), axis=-1).astype(v.dtype)
        p_sel = p[..., :topk * MOBA_BLOCK].reshape(B, MOBA_HEADS, MOBA_QCHUNK, topk, MOBA_BLOCK)
        p_own = p[..., topk * MOBA_BLOCK:]
        return (jnp.einsum('bhqnk,bhqnkd->bhqd', p_sel, v_sel)
                + jnp.einsum('bhqk,bhkd->bhqd', p_own, v_own))

    o = lax.map(chunk, (q_chunks, jnp.arange(nqc)))
    o = o.transpose(1, 0, 3, 2, 4).reshape(B, S, D)
    return o @ w_out


def _rotate(t, cos, sin):
    t1, t2 = jnp.split(t, 2, axis=-1)
    return jnp.concatenate([t1 * cos - t2 * sin, t1 * sin + t2 * cos], axis=-1)


def retention(x, w_in, gn_g, w_out):
    B, S, _ = x.shape
    proj = x @ w_in
    f32 = jnp.float32
    q = _heads(proj[..., :RET_QK], RET_HEADS, RET_DK).astype(f32)
    k = _heads(proj[..., RET_QK:2 * RET_QK], RET_HEADS, RET_DK).astype(f32) * (RET_DK ** -0.5)
    v = _heads(proj[..., 2 * RET_QK:2 * RET_QK + RET_V], RET_HEADS, RET_DV).astype(f32)
    g = proj[..., 2 * RET_QK + RET_V:]
    pos = jnp.arange(S, dtype=f32)
    inv_freq = jnp.exp(-jnp.log(RET_ROPE_BASE) * jnp.arange(0, RET_DK, 2, dtype=f32) / RET_DK)
    ang = pos[:, None] * inv_freq[None, :]
    cos, sin = jnp.cos(ang), jnp.sin(ang)
    q = _rotate(q, cos, sin)
    k = _rotate(k, cos, sin)
    log_gamma = jnp.log(1.0 - jnp.exp2(-5.0 - jnp.arange(RET_HEADS, dtype=f32)))
    n = jnp.arange(RET_CHUNK, dtype=f32)
    diff = n[:, None] - n[None, :]
    d_mask = jnp.where(diff[None] >= 0, jnp.exp(diff[None] * log_gamma[:, None, None]), 0.0)
    xi = jnp.exp((n[None, :] + 1.0) * log_gamma[:, None])
    zeta = jnp.exp((RET_CHUNK - 1.0 - n[None, :]) * log_gamma[:, None])
    g_chunk = jnp.exp(RET_CHUNK * log_gamma)
    nc = S // RET_CHUNK

    def to_chunks(t):
        return t.reshape(B, RET_HEADS, nc, RET_CHUNK, t.shape[-1]).transpose(2, 0, 1, 3, 4)

    def step(R, inp):
        qc, kc, vc = inp
        s = jnp.einsum('bhnd,bhmd->bhnm', qc, kc) * d_mask[None]
        inner = jnp.einsum('bhnm,bhmv->bhnv', s, vc)
        cross = jnp.einsum('bhnd,bhdv->bhnv', qc, R) * xi[None, :, :, None]
        R = g_chunk[None, :, None, None] * R + jnp.einsum('bhmd,bhmv->bhdv', kc, vc * zeta[None, :, :, None])
        return R, inner + cross

    R0 = jnp.zeros((B, RET_HEADS, RET_DK, RET_DV), f32)
    _, o = lax.scan(step, R0, (to_chunks(q), to_chunks(k), to_chunks(v)))
    o = o.transpose(1, 0, 3, 2, 4).reshape(B, S, RET_HEADS, RET_DV)
    mu = jnp.mean(o, axis=-1, keepdims=True)
    var = jnp.mean(jnp.square(o - mu), axis=-1, keepdims=True)
    o = ((o - mu) * lax.rsqrt(var + RET_GN_EPS)).reshape(B, S, RET_V) * gn_g
    o = (jax.nn.silu(g.astype(f32)) * o).astype(x.dtype)
    return o @ w_out


def clamped_swiglu(h):
    glu, lin = h[..., :D_FF], h[..., D_FF:]
    glu = jnp.minimum(glu, SWIGLU_LIMIT)
    lin = jnp.clip(lin, -SWIGLU_LIMIT, SWIGLU_LIMIT)
    return glu * jax.nn.sigmoid(SWIGLU_ALPHA * glu) * (lin + 1.0)


def moe(x2, router_w, router_b, w1, b1, w2, b2):
    T, D = x2.shape
    logits = (x2 @ router_w + router_b).astype(jnp.float32)
    top_v, top_i = lax.top_k(logits, TOP_K)
    gates = jax.nn.softmax(top_v, axis=-1).astype(x2.dtype)
    A = T * TOP_K
    e_flat = top_i.reshape(-1)
    tok_flat = jnp.arange(A, dtype=jnp.int32) // TOP_K
    g_flat = gates.reshape(-1)
    order = jnp.argsort(e_flat)
    e_s, tok_s, g_s = e_flat[order], tok_flat[order], g_flat[order]
    counts = jnp.bincount(e_flat, length=N_EXPERTS)
    starts = jnp.cumsum(counts) - counts
    padded = ((counts + MOE_BLOCK - 1) // MOE_BLOCK) * MOE_BLOCK
    pad_ends = jnp.cumsum(padded)
    pad_starts = pad_ends - padded
    dest = pad_starts[e_s] + (jnp.arange(A, dtype=jnp.int32) - starts[e_s])
    n_blocks = -(-A // MOE_BLOCK) + N_EXPERTS
    P = n_blocks * MOE_BLOCK
    buf_tok = jnp.full((P,), T, jnp.int32).at[dest].set(tok_s)
    buf_g = jnp.zeros((P,), x2.dtype).at[dest].set(g_s)
    blk_start = jnp.arange(n_blocks, dtype=jnp.int32) * MOE_BLOCK
    blk_e = jnp.minimum(jnp.searchsorted(pad_ends, blk_start, side='right'), N_EXPERTS - 1)
    x_pad = jnp.concatenate([x2, jnp.zeros((1, D), x2.dtype)], axis=0)

    def expert_block(args):
        tok, g, e = args
        h = x_pad[tok] @ w1[e] + b1[e]
        y = clamped_swiglu(h) @ w2[e] + b2[e]
        return y * g[:, None]

    y = lax.map(expert_block, (buf_tok.reshape(n_blocks, MOE_BLOCK),
                               buf_g.reshape(n_blocks, MOE_BLOCK), blk_e))
    out = jnp.zeros((T + 1, D), x2.dtype).at[buf_tok].add(y.reshape(P, D))
    return out[:T]


def setup_inputs(seed: int = 0) -> dict:
    key = jax.random.key(seed)
    ks = jax.random.split(key, 20)
    nrm = lambda k, shape: jax.random.normal(k, shape, jnp.float32)
    D = D_MODEL
    x = nrm(ks[0], (BATCH, SEQ, D))
    fox_w_in = nrm(ks[1], (N_FOX, D, FOX_IN)) * D ** -0.5
    fox_b_f = 2.0 + 0.5 * nrm(ks[2], (N_FOX, FOX_HEADS))
    fox_w_out = nrm(ks[3], (N_FOX, D, D)) * (D ** -0.5 * DEEPNORM_BETA)
    moba_w_in = nrm(ks[4], (N_MOBA, D, MOBA_IN)) * D ** -0.5
    moba_w_out = nrm(ks[5], (N_MOBA, D, D)) * (D ** -0.5 * DEEPNORM_BETA)
    ret_w_in = nrm(ks[6], (N_RET, D, RET_IN)) * D ** -0.5
    ret_gn_g = 1.0 + 0.02 * nrm(ks[7], (N_RET, RET_V))
    ret_w_out = nrm(ks[8], (N_RET, RET_V, D)) * (RET_V ** -0.5 * DEEPNORM_BETA)
    ln_g = 1.0 + 0.02 * nrm(ks[9], (DEPTH, 2, D))
    ln_b = 0.02 * nrm(ks[10], (DEPTH, 2, D))
    router_w = nrm(ks[11], (DEPTH, D, N_EXPERTS)) * D ** -0.5
    router_b = 0.01 * nrm(ks[12], (DEPTH, N_EXPERTS))
    moe_w1 = nrm(ks[13], (DEPTH, N_EXPERTS, D, 2 * D_FF)) * D ** -0.5
    moe_b1 = 0.01 * nrm(ks[14], (DEPTH, N_EXPERTS, 2 * D_FF))
    moe_w2 = nrm(ks[15], (DEPTH, N_EXPERTS, D_FF, D)) * (D_FF ** -0.5 * DEEPNORM_BETA)
    moe_b2 = 0.01 * nrm(ks[16], (DEPTH, N_EXPERTS, D))
    return {'x': x, 'fox_w_in': fox_w_in, 'fox_b_f': fox_b_f, 'fox_w_out': fox_w_out,
            'moba_w_in': moba_w_in, 'moba_w_out': moba_w_out,
            'ret_w_in': ret_w_in, 'ret_gn_g': ret_gn_g, 'ret_w_out': ret_w_out,
            'ln_g': ln_g, 'ln_b': ln_b, 'router_w': router_w, 'router_b': router_b,
            'moe_w1': moe_w1, 'moe_b1': moe_b1, 'moe_w2': moe_w2, 'moe_b2': moe_b2}


def reference(x, fox_w_in, fox_b_f, fox_w_out, moba_w_in, moba_w_out, ret_w_in, ret_gn_g,
              ret_w_out, ln_g, ln_b, router_w, router_b, moe_w1, moe_b1, moe_w2, moe_b2):
    B, S, D = x.shape
    for i in range(DEPTH):
        kind = i % N_MIXERS
        j = i // N_MIXERS
        if kind == 0:
            h = forgetting_attention(x, fox_w_in[j], fox_b_f[j], fox_w_out[j])
        elif kind == 1:
            h = moba_attention(x, moba_w_in[j], moba_w_out[j])
        else:
            h = retention(x, ret_w_in[j], ret_gn_g[j], ret_w_out[j])
        x = layer_norm(DEEPNORM_ALPHA * x + h, ln_g[i, 0], ln_b[i, 0])
        y = moe(x.reshape(B * S, D), router_w[i], router_b[i], moe_w1[i], moe_b1[i],
                moe_w2[i], moe_b2[i]).reshape(B, S, D)
        x = layer_norm(DEEPNORM_ALPHA * x + y, ln_g[i, 1], ln_b[i, 1])
    return x
```

```python
import contextlib
import numpy as np
import concourse.bass as bass
import concourse.mybir as mybir
from concourse.bass_utils import run_bass_kernel_spmd

F32 = mybir.dt.float32
BF16 = mybir.dt.bfloat16
AF = mybir.ActivationFunctionType
ALU = mybir.AluOpType
AX = mybir.AxisListType

D = 1024
S = 4096
NB = 4
NOWN = 2048
NT = 16
NE = 32
ALPHA = 8.0 ** 0.25
LN_EPS = 1e-5
OWN_CHUNKS = {0: [0, 3, 4, 7], 1: [1, 2, 5, 6]}

ENGS = ["pe", "act", "dve", "pool", "sp"]
NDMA = 12


class Res:
    __slots__ = ("name", "w", "r")

    def __init__(self, name=""):
        self.name = name
        self.w = None
        self.r = {}


class Prog:
    def __init__(self, nc):
        self.nc = nc
        self.es = contextlib.ExitStack()
        self.scopes = [self.es]
        self.q = {e: [] for e in ENGS}
        self.sem = {}
        for e in ["pe", "act", "dve", "pool"]:
            self.sem[e] = nc.alloc_semaphore("s_" + e)
        self.cnt = {e: 0 for e in ENGS}
        self.dsem = {q: [nc.alloc_semaphore(f"d_{q}{i}") for i in range(NDMA)] for q in ["sp", "pool"]}
        self.dcnt = {"sp": 0, "pool": 0}
        self.waited = {e: {} for e in ENGS}

    def sb(self, name, shape, dtype):
        self.nalloc = getattr(self, "nalloc", 0) + 1
        return self.scopes[-1].enter_context(self.nc.sbuf_tensor(f"sb{self.nalloc}_{name}", list(shape), dtype))

    def ps(self, name, shape, dtype=F32):
        return self.es.enter_context(self.nc.psum_tensor("ps_" + name, list(shape), dtype))

    def push_scope(self):
        self.scopes.append(contextlib.ExitStack())

    def pop_scope(self):
        self.barrier()
        self.scopes.pop().close()

    def _deps(self, reads, writes):
        deps = []
        for r in reads:
            if r.w is not None:
                deps.append(r.w)
        for w in writes:
            if w.w is not None:
                deps.append(w.w)
            deps.extend(w.r.values())
        return deps

    def _emit_waits(self, eng, deps):
        wd = self.waited[eng]
        need = {}
        for (sem, val) in deps:
            k = id(sem)
            if wd.get(k, 0) >= val:
                continue
            if k not in need or need[k][1] < val:
                need[k] = (sem, val)
        for k, (sem, val) in need.items():
            wd[k] = val
            self.q[eng].append(("wait", sem, val))

    def _mark(self, tok, reads, writes):
        sem, val = tok
        k = id(sem)
        for r in reads:
            if k not in r.r or r.r[k][1] < val:
                r.r[k] = tok
        for w in writes:
            w.w = tok
            w.r = {}

    def op(self, eng, fn, reads=(), writes=()):
        deps = self._deps(reads, writes)
        if eng == "pe":
            deps = [d for d in deps if d[0] is not self.sem["pe"]]
        self._emit_waits(eng, deps)
        self.cnt[eng] += 1
        tok = (self.sem[eng], self.cnt[eng])
        self.q[eng].append(("op", fn, self.sem[eng]))
        self._mark(tok, reads, writes)
        return tok

    def dma(self, queue, fn, reads=(), writes=()):
        deps = self._deps(reads, writes)
        k = self.dcnt[queue]
        self.dcnt[queue] += 1
        sem = self.dsem[queue][k % NDMA]
        prev = 16 * (k // NDMA)
        if prev > 0:
            deps.append((sem, prev))
        self._emit_waits(queue, deps)
        tok = (sem, prev + 16)
        self.q[queue].append(("dma", fn, sem))
        self._mark(tok, reads, writes)
        return tok

    def all_tokens(self):
        toks = [(self.sem[e], self.cnt[e]) for e in ["pe", "act", "dve", "pool"] if self.cnt[e] > 0]
        for q in ["sp", "pool"]:
            k = self.dcnt[q]
            for i in range(min(k, NDMA)):
                n_i = (k - 1 - i) // NDMA + 1
                toks.append((self.dsem[q][i], 16 * n_i))
        return toks

    def barrier(self):
        toks = self.all_tokens()
        for e in ENGS:
            self._emit_waits(e, toks)

    def finish(self):
        self._emit_waits("sp", self.all_tokens())
        nc = self.nc
        qs = self.q

        def run(engobj, items):
            for it in items:
                if it[0] == "wait":
                    engobj.wait_ge(it[1], it[2])
                elif it[0] == "op":
                    it[1](engobj).then_inc(it[2], 1)
                else:
                    it[1](engobj).then_inc(it[2], 16)

        with nc.Block() as block:
            @block.sync
            def _(e):
                run(e, qs["sp"])

            @block.tensor
            def _(e):
                run(e, qs["pe"])

            @block.scalar
            def _(e):
                run(e, qs["act"])

            @block.vector
            def _(e):
                run(e, qs["dve"])

            @block.gpsimd
            def _(e):
                run(e, qs["pool"])
        self.es.close()


def emit_layernorm(P, C, src, src_res, dst, dst_res, g, b, eng2="pool"):
    k = C["ln_k"] = (C.get("ln_k", 0) + 1) % 2
    st, mv, sd, r = C["ln_st"][k], C["ln_mv"][k], C["ln_sd"][k], C["ln_res"][k]
    for hlf in range(2):
        P.op("dve", lambda e, hlf=hlf: e.bn_stats(out=st[:, hlf * 6:(hlf + 1) * 6],
                                                   in_=src[:, hlf * 512:(hlf + 1) * 512]),
             reads=[src_res], writes=[r])
    P.op("dve", lambda e: e.bn_aggr(out=mv[:, 0:2], in_=st[:, 0:12]), reads=[r], writes=[r])
    P.op("dve", lambda e: e.tensor_scalar(out=sd[:, 0:1], in0=mv[:, 1:2], scalar1=LN_EPS, scalar2=None,
                                          op0=ALU.add), reads=[r], writes=[r])
    P.op("act", lambda e: e.activation(out=sd[:, 1:2], in_=sd[:, 0:1], func=AF.Sqrt), reads=[r], writes=[r])
    P.op("dve", lambda e: e.reciprocal(out=sd[:, 2:3], in_=sd[:, 1:2]), reads=[r], writes=[r])
    P.op("dve", lambda e: e.tensor_scalar(out=dst, in0=src, scalar1=mv[:, 0:1], scalar2=sd[:, 2:3],
                                          op0=ALU.subtract, op1=ALU.mult),
         reads=[src_res, r], writes=[dst_res])
    P.op(eng2, lambda e: e.tensor_tensor(out=dst, in0=dst, in1=g, op=ALU.mult),
         reads=[dst_res, C["const_res"]], writes=[dst_res])
    P.op(eng2, lambda e: e.tensor_tensor(out=dst, in0=dst, in1=b, op=ALU.add),
         reads=[dst_res, C["const_res"]], writes=[dst_res])


def emit_moe(P, C, dr, ne_run=NE, stage=9):
    xm, xm_res = C["xm"], C["xm_res"]
    bank, bank_res = C["bank"], C["bank_res"]
    ident = C["ident"]
    cres = C["const_res"]
    P.push_scope()
    rw = load_const(P, C, "rw", dr["rw"].rearrange("(kc q) n -> q kc n", q=128), [128, 8, NE])
    rbB = load_const(P, C, "rbB", dr["rbB"], [128, NE])
    b1T = load_const(P, C, "b1T", dr["b1T"], [128, NE, 16])
    b2s = load_const(P, C, "b2s", dr["b2s"], [NE, D])
    Um = load_const(P, C, "Um", dr["Umat"], [128, 128])
    iota = load_const(P, C, "iota", dr["iota"], [128, 512])
    jp = load_const(P, C, "jp", dr["jp"], [128, NT, 2])
    onesf = P.sb("onesf", [128, 128], F32)
    P.op("dve", lambda e: e.memset(onesf[:], 1.0), writes=[cres])
    xb = P.sb("xb", [128, NT, D], BF16)
    xb_res = [Res() for _ in range(NT)]
    xTf = P.sb("xTf", [128, 8, 128], F32)
    xTf_res = [Res(), Res()]
    G = P.sb("G", [128, NT, NE], F32)
    M = P.sb("M", [128, NT, NE], F32)
    R = P.sb("R", [128, NT, NE], F32)
    G_res = [Res() for _ in range(NT)]
    R_res = [Res() for _ in range(NT)]
    GTt = P.sb("GTt", [NE, 128], F32)
    GTt_res = Res()
    cm = P.sb("cm", [128, NE], F32)
    cm_res = Res()
    TB = P.sb("TB", [128, NT, 4], BF16)
    TBf = P.sb("TBf", [128, NT], F32)
    TB_res = Res()
    lg = P.sb("lg", [128, NE], F32)
    top8 = P.sb("top8", [128, 8], F32)
    rt = P.sb("rt", [128, 4], F32)
    ex = P.sb("ex", [128, NE], F32)
    r_res = Res()
    P.op("dve", lambda e: e.tensor_copy(out=TB[:, :, 0:2], in_=jp[:]), reads=[cres], writes=[TB_res])

    def route_a(t):
        lb = 2 if t % 2 == 0 else 7
        P.op("pool", lambda e, t=t: e.tensor_copy(out=xb[:, t, :], in_=xm[:, t, :]), reads=[xm_res[t]], writes=[xb_res[t]])
        for hb in range(2):
            bk = hb
            for j in range(4):
                dc = hb * 4 + j
                P.op("pe", lambda e, t=t, dc=dc, j=j, bk=bk: e.transpose(
                    out=bank[:, bk, j * 128:(j + 1) * 128], in_=xm[:, t, dc * 128:(dc + 1) * 128], identity=ident[:]),
                    reads=[xm_res[t], cres], writes=[bank_res[bk]])
            P.op("act", lambda e, hb=hb, bk=bk: e.activation(
                out=xTf[:, hb * 4:(hb + 1) * 4, :], in_=bank[:, bk, :].rearrange("p (j c) -> p j c", j=4),
                func=AF.Copy), reads=[bank_res[bk]], writes=[xTf_res[hb]])
        for dc in range(8):
            P.op("pe", lambda e, dc=dc, lb=lb: e.matmul(bank[:, lb, 0:NE], xTf[:, dc, :], rw[:, dc, :],
                                                       start=(dc == 0), stop=(dc == 7)),
                 reads=[xTf_res[dc // 4], cres], writes=[bank_res[lb]])

    def route_b(t):
        lb = 2 if t % 2 == 0 else 7
        P.op("dve", lambda e, lb=lb: e.tensor_tensor(out=lg[:], in0=bank[:, lb, 0:NE], in1=rbB[:], op=ALU.add),
             reads=[bank_res[lb], cres], writes=[r_res])
        P.op("dve", lambda e: e.max(out=top8[:], in_=lg[:]), reads=[r_res], writes=[r_res])
        P.op("dve", lambda e, t=t: e.tensor_scalar(out=M[:, t, :], in0=lg[:], scalar1=top8[:, 3:4], scalar2=None,
                                                   op0=ALU.is_ge), reads=[r_res], writes=[r_res, G_res[t]])
        P.op("dve", lambda e: e.tensor_scalar(out=rt[:, 0:1], in0=top8[:, 0:1], scalar1=-1.0, scalar2=None,
                                              op0=ALU.mult), reads=[r_res], writes=[r_res])
        P.op("act", lambda e: e.activation(out=ex[:], in_=lg[:], func=AF.Exp, bias=rt[:, 0:1], scale=1.0),
             reads=[r_res], writes=[r_res])
        P.op("dve", lambda e, t=t: e.tensor_tensor(out=ex[:], in0=ex[:], in1=M[:, t, :], op=ALU.mult),
             reads=[r_res, G_res[t]], writes=[r_res])
        P.op("dve", lambda e: e.reduce_sum(out=rt[:, 1:2], in_=ex[:], axis=AX.X), reads=[r_res], writes=[r_res])
        P.op("dve", lambda e: e.reciprocal(out=rt[:, 2:3], in_=rt[:, 1:2]), reads=[r_res], writes=[r_res])
        P.op("dve", lambda e, t=t: e.tensor_scalar(out=G[:, t, :], in0=ex[:], scalar1=rt[:, 2:3], scalar2=None,
                                                   op0=ALU.mult), reads=[r_res, G_res[t]], writes=[G_res[t]])
        jg = t % 4
        if jg > 0:
            P.op("pe", lambda e: e.matmul(bank[:, 4, 0:NE], onesf[:], cm[:], start=True, stop=False),
                 reads=[cm_res, cres], writes=[bank_res[4]])
        P.op("pe", lambda e, t=t, jg=jg: e.matmul(bank[:, 4, 0:NE], Um[:], M[:, t, :], start=(jg == 0), stop=True),
             reads=[G_res[t], cres], writes=[bank_res[4]])
        P.op("act", lambda e, t=t: e.activation(out=R[:, t, :], in_=bank[:, 4, 0:NE], func=AF.Copy),
             reads=[bank_res[4]], writes=[R_res[t]])
        if jg == 0:
            P.op("dve", lambda e, t=t: e.tensor_copy(out=cm[:], in_=M[:, t, :]), reads=[G_res[t]], writes=[cm_res])
        elif jg < 3:
            P.op("dve", lambda e, t=t: e.tensor_tensor(out=cm[:], in0=cm[:], in1=M[:, t, :], op=ALU.add),
                 reads=[G_res[t], cm_res], writes=[cm_res])
        P.op("pe", lambda e, t=t: e.transpose(out=bank[0:NE, 3, 0:128], in_=G[:, t, :], identity=ident[:]),
             reads=[G_res[t], cres], writes=[bank_res[3]])
        P.op("act", lambda e: e.activation(out=GTt[:], in_=bank[0:NE, 3, 0:128], func=AF.Copy),
             reads=[bank_res[3]], writes=[GTt_res])
        for p in range(2):
            bk = 5 + p
            P.op("pe", lambda e, bk=bk, p=p: e.matmul(bank[:, bk, :], GTt[:], b2s[:, p * 512:(p + 1) * 512], start=True, stop=True),
                 reads=[GTt_res, cres], writes=[bank_res[bk]])
            P.op("dve", lambda e, bk=bk, t=t, p=p: e.scalar_tensor_tensor(
                out=xm[:, t, p * 512:(p + 1) * 512], in0=xm[:, t, p * 512:(p + 1) * 512], scalar=ALPHA, in1=bank[:, bk, :],
                op0=ALU.mult, op1=ALU.add),
                reads=[bank_res[bk], xm_res[t], xb_res[t]], writes=[xm_res[t]])

    route_a(0)
    for t in range(NT):
        if t + 1 < NT:
            route_a(t + 1)
        route_b(t)

    NRING = 4
    ring = [P.sb(f"ring{i}", [128, 8, 512], BF16) for i in range(NRING)]
    ring_res = [Res() for _ in range(NRING)]
    xgT = [P.sb(f"xgT{i}", [128, 8, 512], BF16) for i in range(2)]
    xgT_res = [[Res() for _ in range(4)] for _ in range(2)]
    Sel = P.sb("Sel", [128, NT, 128], BF16)
    Sel_res = [Res() for _ in range(4)]
    SelGT = [P.sb(f"SelGT{i}", [128, 4, 512], BF16) for i in range(2)]
    SelGT_res = [[Res() for _ in range(4)] for _ in range(2)]
    tkg = P.sb("tkg", [128, 2], F32)
    tkg_res = Res()
    aT = P.sb("aT", [128, 8, 512], BF16)
    aT_res = [Res() for _ in range(8)]
    ysb = P.sb("ysb", [128, 4, D], BF16)
    ysb_res = [Res() for _ in range(4)]
    tb = [P.sb(f"tb{i}", [128, 512], F32) for i in range(2)]
    tb_res = [Res() for _ in range(2)]
    sg = [P.sb(f"sg{i}", [128, 512], BF16) for i in range(2)]
    sg_res = [Res() for _ in range(2)]
    w1, w2 = dr["w1"], dr["w2"]
    if stage < 4:
        ne_run = 0
    pieces = []
    for ex_i in range(ne_run):
        for p in range(4):
            pieces.append((ex_i, "w1", p))
        for p in range(2):
            pieces.append((ex_i, "w2", p))
    st = {"next_load": 0, "sel": 0, "hb": 0, "tb": 0, "sg": 0, "yb": 0, "cb": 0}

    def load_piece():
        idx = st["next_load"]
        if idx >= len(pieces):
            return
        st["next_load"] += 1
        ex_i, kind, p = pieces[idx]
        slot = idx % NRING
        src = (w1 if kind == "w1" else w2)[ex_i, :, p * 512:(p + 1) * 512].rearrange("(kc q) c -> q kc c", q=128)
        P.dma("pool", lambda e, slot=slot, src=src: e.dma_start(out=ring[slot][:], in_=src), writes=[ring_res[slot]])

    def sel_prep(ex_i):
        P.op("dve", lambda e: e.tensor_copy(out=TB[:, :, 2], in_=G[:, :, ex_i]), reads=G_res, writes=[TB_res])
        P.op("dve", lambda e: e.tensor_tensor(out=TBf[:], in0=G[:, :, ex_i], in1=TB[:, :, 2], op=ALU.subtract),
             reads=G_res + [TB_res], writes=[TB_res])
        P.op("dve", lambda e: e.tensor_copy(out=TB[:, :, 3], in_=TBf[:]), reads=[TB_res], writes=[TB_res])

    def sel_build(ex_i, t):
        P.op("dve", lambda e: e.tensor_scalar(
            out=Sel[:, t, :], in0=iota[:, 0:128], scalar1=R[:, t, ex_i:ex_i + 1], scalar2=M[:, t, ex_i:ex_i + 1],
            op0=ALU.is_equal, op1=ALU.mult), reads=[R_res[t], G_res[t], cres], writes=[Sel_res[t // 4]])

    def gather(ex_i):
        xg, xg_res = xgT[ex_i % 2], xgT_res[ex_i % 2]
        sgt, sgt_res = SelGT[ex_i % 2], SelGT_res[ex_i % 2]
        for g in range(4):
            for hb in range(2):
                bk = hb
                for jj in range(4):
                    dc = hb * 4 + jj
                    for j in range(4):
                        t = g * 4 + j
                        P.op("pe", lambda e, bk=bk, jj=jj, dc=dc, j=j, t=t: e.matmul(
                            bank[:, bk, jj * 128:(jj + 1) * 128], xb[:, t, dc * 128:(dc + 1) * 128], Sel[:, t, :],
                            start=(j == 0), stop=(j == 3)),
                            reads=[xb_res[t], Sel_res[g]], writes=[bank_res[bk]])
                P.op("act", lambda e, hb=hb, bk=bk, g=g, xg=xg: e.activation(
                    out=xg[:, hb * 4:(hb + 1) * 4, g * 128:(g + 1) * 128], in_=bank[:, bk, :].rearrange("p (j c) -> p j c", j=4),
                    func=AF.Copy), reads=[bank_res[bk]], writes=[xg_res[g]])
            for j in range(4):
                t = g * 4 + j
                P.op("pe", lambda e, j=j, t=t: e.matmul(bank[:, 7, 0:4], Sel[:, t, :], TB[:, t, :],
                                                        start=(j == 0), stop=(j == 3)),
                     reads=[Sel_res[g], TB_res], writes=[bank_res[7]])
            P.op("dve", lambda e: e.tensor_scalar(out=tkg[:, 0:1], in0=bank[:, 7, 0:1], scalar1=128.0, scalar2=bank[:, 7, 1:2],
                                                  op0=ALU.mult, op1=ALU.add), reads=[bank_res[7]], writes=[tkg_res])
            P.op("dve", lambda e: e.tensor_scalar(out=tkg[:, 1:2], in0=bank[:, 7, 2:3], scalar1=bank[:, 7, 3:4], scalar2=None,
                                                  op0=ALU.add), reads=[bank_res[7]], writes=[tkg_res])
            P.op("dve", lambda e, g=g, sgt=sgt: e.tensor_scalar(out=sgt[:, g, :], in0=iota[:], scalar1=tkg[:, 0:1], scalar2=tkg[:, 1:2],
                                                              op0=ALU.is_equal, op1=ALU.mult),
                 reads=[tkg_res, cres], writes=[sgt_res[g]])

    def ffn1(ex_i, base):
        xg, xg_res = xgT[ex_i % 2], xgT_res[ex_i % 2]
        nxt = ex_i + 1 if ex_i + 1 < ne_run else None
        if nxt is not None:
            sel_prep(nxt)
        for p in range(4):
            slot = (base + p) % NRING
            for j in range(4):
                fc = p * 4 + j
                bk = 4 + (st["hb"] % 3)
                st["hb"] += 1
                for dc in range(8):
                    P.op("pe", lambda e, bk=bk, slot=slot, j=j, dc=dc, xg=xg: e.matmul(
                        bank[:, bk, :], ring[slot][:, dc, j * 128:(j + 1) * 128], xg[:, dc, :], start=(dc == 0), stop=(dc == 7)),
                        reads=[ring_res[slot]] + xg_res, writes=[bank_res[bk]])
                ti = st["tb"] % 2
                st["tb"] += 1
                b1col = b1T[:, ex_i, fc:fc + 1]
                P.op("dve", lambda e, ti=ti, bk=bk, b1col=b1col: e.tensor_scalar(
                    out=tb[ti][:], in0=bank[:, bk, :], scalar1=b1col, scalar2=7.0, op0=ALU.add, op1=ALU.min),
                    reads=[bank_res[bk], cres], writes=[tb_res[ti]])
                if fc < 8:
                    si = st["sg"] % 2
                    st["sg"] += 1
                    P.op("act", lambda e, ti=ti, si=si: e.activation(out=sg[si][:], in_=tb[ti][:], func=AF.Sigmoid, scale=1.702),
                         reads=[tb_res[ti]], writes=[sg_res[si]])
                    P.op("pool", lambda e, ti=ti, si=si, fc=fc: e.tensor_tensor(out=aT[:, fc, :], in0=tb[ti][:], in1=sg[si][:], op=ALU.mult),
                         reads=[tb_res[ti], sg_res[si]], writes=[aT_res[fc]])
                else:
                    fg = fc - 8
                    P.op("dve", lambda e, ti=ti: e.tensor_scalar(out=tb[ti][:], in0=tb[ti][:], scalar1=-7.0, scalar2=1.0,
                                                                 op0=ALU.max, op1=ALU.add), reads=[tb_res[ti]], writes=[tb_res[ti]])
                    P.op("pool", lambda e, ti=ti, fg=fg: e.tensor_tensor(out=aT[:, fg, :], in0=aT[:, fg, :], in1=tb[ti][:], op=ALU.mult),
                         reads=[tb_res[ti], aT_res[fg]], writes=[aT_res[fg]])
                if nxt is not None:
                    sel_build(nxt, fc)
            load_piece()

    def ffn2(ex_i, base):
        for p in range(2):
            slot = (base + 4 + p) % NRING
            for g in range(4):
                bk = 2 + (st["yb"] % 2)
                st["yb"] += 1
                for fc in range(8):
                    P.op("pe", lambda e, bk=bk, fc=fc, g=g, slot=slot: e.matmul(
                        bank[:, bk, :], aT[:, fc, g * 128:(g + 1) * 128], ring[slot][:, fc, :], start=(fc == 0), stop=(fc == 7)),
                        reads=[ring_res[slot], aT_res[fc]], writes=[bank_res[bk]])
                P.op("act", lambda e, bk=bk, g=g, p=p: e.activation(out=ysb[:, g, p * 512:(p + 1) * 512], in_=bank[:, bk, :], func=AF.Copy),
                     reads=[bank_res[bk]], writes=[ysb_res[g]])
            load_piece()

    def combine(ex_i):
        sgt, sgt_res = SelGT[ex_i % 2], SelGT_res[ex_i % 2]
        for g in range(4):
            for j in range(4):
                t = g * 4 + j
                for hf in range(2):
                    bk = st["cb"] % 4
                    st["cb"] += 1
                    P.op("pe", lambda e, bk=bk, g=g, j=j, hf=hf, sgt=sgt: e.matmul(
                        bank[:, bk, :], sgt[:, g, j * 128:(j + 1) * 128], ysb[:, g, hf * 512:(hf + 1) * 512], start=True, stop=True),
                        reads=[sgt_res[g], ysb_res[g]], writes=[bank_res[bk]])
                    P.op("dve", lambda e, bk=bk, t=t, hf=hf: e.tensor_tensor(
                        out=xm[:, t, hf * 512:(hf + 1) * 512], in0=bank[:, bk, :], in1=xm[:, t, hf * 512:(hf + 1) * 512], op=ALU.add),
                        reads=[bank_res[bk], xm_res[t]], writes=[xm_res[t]])

    for _ in range(3):
        load_piece()
    if ne_run > 0:
        sel_prep(0)
        for t in range(NT):
            sel_build(0, t)
        gather(0)
    for ex_i in range(ne_run):
        ffn1(ex_i, ex_i * 6)
        if ex_i + 1 < ne_run:
            gather(ex_i + 1)
        ffn2(ex_i, ex_i * 6)
        combine(ex_i)
    P.pop_scope()

    P.push_scope()
    lnp = load_const(P, C, "lnp", dr["lnp"][:, 2:4, :], [128, 2, D])
    xTst = [P.sb(f"xTst{i}", [128, 8, 128], BF16) for i in range(2)]
    xTst_res = [Res(), Res()]
    for tt in range(NT):
        if stage >= 6:
            emit_layernorm(P, C, xm[:, tt, :], xm_res[tt], xm[:, tt, :], xm_res[tt], lnp[:, 0, :], lnp[:, 1, :])
        P.dma("sp", lambda e, tt=tt: e.dma_start(out=dr["xout"][tt * 128:(tt + 1) * 128, :], in_=xm[:, tt, :]),
              reads=[xm_res[tt]], writes=_r(dr, "xout_res"))
        if dr.get("xTnext") is not None:
            k = tt % 2
            for hb in range(2):
                bk = 2 + hb
                for j in range(4):
                    dc = hb * 4 + j
                    P.op("pe", lambda e, tt=tt, dc=dc, j=j, bk=bk: e.transpose(
                        out=bank[:, bk, j * 128:(j + 1) * 128], in_=xm[:, tt, dc * 128:(dc + 1) * 128], identity=ident[:]),
                        reads=[xm_res[tt], cres], writes=[bank_res[bk]])
                P.op("act", lambda e, hb=hb, bk=bk, k=k: e.activation(
                    out=xTst[k][:, hb * 4:(hb + 1) * 4, :], in_=bank[:, bk, :].rearrange("p (j c) -> p j c", j=4),
                    func=AF.Copy), reads=[bank_res[bk]], writes=[xTst_res[k]])
            c0 = dr["xTnext_col0"] + tt * 128
            dst = dr["xTnext"][:, c0:c0 + 128].rearrange("(dc p) t -> p dc t", p=128)
            P.dma("sp", lambda e, k=k, dst=dst: e.dma_start(out=dst, in_=xTst[k][:]),
                  reads=[xTst_res[k]], writes=_r(dr, "xTnext_res"))
    P.pop_scope()


NEGBIG = -30000.0


def _r(dr, key):
    r = dr.get(key)
    return [] if r is None else (list(r) if isinstance(r, (list, tuple)) else [r])
HD = 128


def key_chunks(ci):
    out = []
    for c in range(ci + 1):
        for m in range(4):
            out.append((4 * c + m, c == ci))
    for c in range(ci + 1):
        for m in range(4):
            out.append((16 + 4 * c + m, c == ci))
    return out


def mask_index(ci, jl):
    return ci * 8 + (jl % 4 if jl < 16 else 4 + jl % 4)


def emit_mixer_common(P, C, dr):
    cres = C["const_res"]
    qposB = P.sb("qposB", [128, NOWN], F32)
    kposc = P.sb("kposc", [128, 32], F32)
    P.dma("sp", lambda e: e.dma_start(out=qposB[:], in_=dr["qposB"]), writes=[cres])
    P.dma("sp", lambda e: e.dma_start(out=kposc[:], in_=dr["kposc"]), writes=[cres])
    identb = P.sb("identb", [128, 128], BF16)
    onesb = P.sb("onesb", [128, 128], BF16)
    P.op("dve", lambda e: e.tensor_copy(out=identb[:], in_=C["ident"][:]), reads=[cres], writes=[cres])
    P.op("dve", lambda e: e.memset(onesb[:], 1.0), writes=[cres])
    C.update(qposB=qposB, kposc=kposc, identb=identb, onesb=onesb)
    xm, xm_res = C["xm"], C["xm_res"]
    for t in range(NT):
        P.dma("sp", lambda e, t=t: e.dma_start(out=xm[:, t, :], in_=dr["xown"][t * 128:(t + 1) * 128, :]),
              reads=_r(dr, "xown_res"), writes=[xm_res[t]])
        P.op("pool", lambda e, t=t: e.tensor_scalar(out=xm[:, t, :], in0=xm[:, t, :], scalar1=ALPHA, scalar2=1.0,
                                                    op0=ALU.mult, op1=ALU.mult),
             reads=[xm_res[t]], writes=[xm_res[t]])


def emit_masks(P, C):
    C["maskT"] = [P.sb(f"maskT{i}", [128, 8, 512], BF16) for i in range(2)]
    C["mask_res"] = [Res("mask0"), Res("mask1")]
    C["mask_k"] = 0


def gen_masks(P, C, ci):
    i = C["mask_k"] % 2
    C["mask_k"] += 1
    qposB, kposc = C["qposB"], C["kposc"]
    for (jl, masked) in key_chunks(ci):
        if not masked:
            continue
        mi = mask_index(ci, jl) % 8
        P.op("dve", lambda e, ci=ci, jl=jl, mi=mi, i=i: e.tensor_scalar(
            out=C["maskT"][i][:, mi, :], in0=qposB[:, ci * 512:(ci + 1) * 512], scalar1=kposc[:, jl:jl + 1],
            scalar2=NEGBIG, op0=ALU.is_lt, op1=ALU.mult),
            reads=[C["const_res"]], writes=[C["mask_res"][i]])
    return C["maskT"][i], C["mask_res"][i]


class SlabLoader:
    def __init__(self, P, dr, n=2):
        self.P, self.dr, self.n = P, dr, n
        self.buf = [P.sb(f"slab{i}", [128, 8, 512], BF16) for i in range(n)]
        self.res = [Res(f"slab{i}") for i in range(n)]
        self.k = 0

    def load(self, s):
        i = self.k % self.n
        self.k += 1
        pb = self.dr.get("slab_map", list(range(8)))[s]
        src = self.dr["xT"][:, pb * 512:(pb + 1) * 512].rearrange("(dc p) t -> p dc t", p=128)
        self.P.dma("pool", lambda e, i=i, src=src: e.dma_start(out=self.buf[i][:], in_=src),
                   reads=_r(self.dr, "xT_res"), writes=[self.res[i]])
        return self.buf[i], self.res[i]


def emit_attention_mixer(P, C, dr, kind):
    bank, bank_res = C["bank"], C["bank_res"]
    cres = C["const_res"]
    xm, xm_res = C["xm"], C["xm_res"]
    identb, onesb = C["identb"], C["onesb"]
    ident = C["ident"]
    w_in, w_out = dr["w_in"], dr["w_out"]
    NH = 8
    scale = HD ** -0.5
    slabs = SlabLoader(P, dr)
    wh = [P.sb(f"wh{i}", [128, 8, 3, 128], BF16) for i in range(2)]
    wh_res = [Res() for _ in range(2)]
    wo = [P.sb(f"wo{i}", [128, D], BF16) for i in range(2)]
    wo_res = [Res() for _ in range(2)]
    kT = P.sb("kT", [128, S], BF16)
    kT_res = [Res() for _ in range(8)]
    vv = P.sb("vv", [128, 32, 128], BF16)
    vv_res = [Res() for _ in range(8)]
    qT = P.sb("qT", [128, NOWN], BF16)
    qT_res = [Res() for _ in range(4)]
    oT = P.sb("oT", [128, NOWN], BF16)
    oT_res = [Res() for _ in range(4)]
    pT = [P.sb(f"pT{i}", [128, 512], BF16) for i in range(3)]
    pT_res = [Res() for _ in range(3)]
    rs = P.sb("rs", [128, 512], F32)
    rs_res = Res()
    pacc = [P.sb(f"pacc{i}", [128, 512], F32) for i in range(2)]
    pacc_res = [Res(), Res()]
    onesf = P.sb("onesf", [128, 128], F32)
    P.op("dve", lambda e: e.memset(onesf[:], 1.0), writes=[cres])

    def load_head_weights(h):
        i = h % 2
        for j in range(3):
            src = w_in[:, j * D + h * HD: j * D + (h + 1) * HD].rearrange("(dc p) c -> p dc c", p=128)
            P.dma("pool", lambda e, i=i, j=j, src=src: e.dma_start(out=wh[i][:, :, j, :], in_=src), writes=[wh_res[i]])
        src = w_out[h * HD:(h + 1) * HD, :]
        P.dma("pool", lambda e, i=i, src=src: e.dma_start(out=wo[i][:], in_=src), writes=[wo_res[i]])

    if kind == "fox":
        wf = P.sb("wf", [128, 8, 8], BF16)
        P.dma("pool", lambda e: e.dma_start(out=wf[:], in_=w_in[:, 3 * D:3 * D + 8].rearrange("(dc p) c -> p dc c", p=128)),
              writes=[cres])
        bfn = P.sb("bfn", [8, 1], F32)
        P.dma("sp", lambda e: e.dma_start(out=bfn[:], in_=dr["b_f"]), writes=[cres])
        P.op("dve", lambda e: e.tensor_scalar(out=bfn[:], in0=bfn[:], scalar1=-1.0, scalar2=None, op0=ALU.mult),
             reads=[cres], writes=[cres])
        LcT = P.sb("LcT", [8, 64], F32)
        P.dma("sp", lambda e: e.dma_start(out=LcT[:], in_=dr["Lc"]), writes=[cres])
        selh = P.sb("selh", [24, 8, 128], BF16)
        P.dma("pool", lambda e: e.dma_start(out=selh[:], in_=dr["selh"]), writes=[cres])
        ones8 = P.sb("ones8", [8, 512], F32)
        P.op("dve", lambda e: e.memset(ones8[:], 1.0), writes=[cres])
        csp = P.sb("csp", [8, S], F32)
        csp_res = Res()
        e1 = P.sb("e1", [8, 512], F32)
        e1_res = Res()
        tot = P.sb("tot", [8, 8], F32)
        off = P.sb("off", [8, 8], F32)
        tmp8 = P.sb("tmp8", [8, 8], F32)
        for s in range(8):
            sl, sl_res = slabs.load(s)
            for dc in range(8):
                P.op("pe", lambda e, dc=dc, sl=sl: e.matmul(bank[0:8, 0, :], wf[:, dc, :], sl[:, dc, :],
                                                            start=(dc == 0), stop=(dc == 7)),
                     reads=[sl_res, cres], writes=[bank_res[0]])
            P.op("act", lambda e: e.activation(out=e1[:], in_=bank[0:8, 0, :], func=AF.Exp, bias=bfn[:, 0:1], scale=-1.0),
                 reads=[bank_res[0], cres], writes=[e1_res])
            P.op("act", lambda e: e.activation(out=e1[:], in_=e1[:], func=AF.Ln, bias=1.0, scale=1.0),
                 reads=[e1_res], writes=[e1_res])
            P.op("dve", lambda e, s=s: e.tensor_tensor_scan(out=csp[:, s * 512:(s + 1) * 512], data0=ones8[:], data1=e1[:],
                                                           initial=0.0, op0=ALU.mult, op1=ALU.add),
                 reads=[e1_res, cres], writes=[csp_res])
            P.op("dve", lambda e, s=s: e.tensor_copy(out=tot[:, s:s + 1], in_=csp[:, (s + 1) * 512 - 1:(s + 1) * 512]),
                 reads=[csp_res], writes=[csp_res])
        for ci in range(8):
            P.op("dve", lambda e, ci=ci: e.tensor_tensor(out=tmp8[:], in0=tot[:], in1=LcT[:, ci * 8:(ci + 1) * 8], op=ALU.mult),
                 reads=[csp_res, cres], writes=[csp_res])
            P.op("dve", lambda e, ci=ci: e.reduce_sum(out=off[:, ci:ci + 1], in_=tmp8[:], axis=AX.X),
                 reads=[csp_res], writes=[csp_res])
        for ci in range(8):
            P.op("dve", lambda e, ci=ci: e.tensor_scalar(out=csp[:, ci * 512:(ci + 1) * 512], in0=csp[:, ci * 512:(ci + 1) * 512],
                                                         scalar1=off[:, ci:ci + 1], scalar2=None, op0=ALU.add),
                 reads=[csp_res], writes=[csp_res])
        Ck = P.sb("Ck", [128, 32, 8], F32)
        for g in range(2):
            for j in range(16):
                jl = g * 16 + j
                P.op("pe", lambda e, jl=jl, j=j: e.transpose(out=bank[:, 1, j * 8:(j + 1) * 8], in_=csp[:, jl * 128:(jl + 1) * 128],
                                                             identity=ident[0:8, 0:8]),
                     reads=[csp_res, cres], writes=[bank_res[1]])
            P.op("act", lambda e, g=g: e.activation(out=Ck[:, g * 16:(g + 1) * 16, :],
                                                    in_=bank[:, 1, 0:128].rearrange("p (j h) -> p j h", h=8), func=AF.Copy),
                 reads=[bank_res[1]], writes=[csp_res])
        c3 = P.sb("c3", [24, NOWN], BF16)
        c3p = [P.sb(f"c3p{i}", [8, 512], BF16) for i in range(3)]
        r1 = P.sb("r1", [8, 512], F32)
        r2 = P.sb("r2", [8, 512], F32)
        for qc in range(4):
            cs = csp[:, qc * 512:(qc + 1) * 512]
            P.op("dve", lambda e, cs=cs: e.tensor_scalar(out=c3p[0][:], in0=cs, scalar1=-1.0, scalar2=None, op0=ALU.mult),
                 reads=[csp_res], writes=[csp_res])
            P.op("dve", lambda e, cs=cs: e.tensor_tensor(out=r1[:], in0=cs, in1=c3p[0][:], op=ALU.add),
                 reads=[csp_res], writes=[csp_res])
            P.op("dve", lambda e: e.tensor_scalar(out=c3p[1][:], in0=r1[:], scalar1=-1.0, scalar2=None, op0=ALU.mult),
                 reads=[csp_res], writes=[csp_res])
            P.op("dve", lambda e: e.tensor_tensor(out=r2[:], in0=r1[:], in1=c3p[1][:], op=ALU.add),
                 reads=[csp_res], writes=[csp_res])
            P.op("dve", lambda e: e.tensor_scalar(out=c3p[2][:], in0=r2[:], scalar1=-1.0, scalar2=None, op0=ALU.mult),
                 reads=[csp_res], writes=[csp_res])
            for i in range(3):
                P.dma("sp", lambda e, i=i, qc=qc: e.dma_start(out=c3[i * 8:(i + 1) * 8, qc * 512:(qc + 1) * 512], in_=c3p[i][:]),
                      reads=[csp_res], writes=[csp_res])
        fox_res = csp_res
    else:
        esel = P.sb("esel", [16, 16, 128], BF16)
        P.dma("pool", lambda e: e.dma_start(out=esel[:], in_=dr["esel"]), writes=[cres])
        bposB = P.sb("bposB", [128, 16], F32)
        ownb = P.sb("ownb", [128, 16], F32)
        P.dma("sp", lambda e: e.dma_start(out=bposB[:], in_=dr["bposB"]), writes=[cres])
        P.dma("sp", lambda e: e.dma_start(out=ownb[:], in_=dr["ownb"]), writes=[cres])
        kms = P.sb("kms", [128, 16], F32)
        kmT = P.sb("kmT", [128, 16], BF16)
        km_res = Res()
        selbT = P.sb("selbT", [16, NOWN], BF16)
        selb_res = Res()
        gsc = {n: P.sb("g_" + n, [128, 16], F32) for n in ["past", "nb", "gm", "sel", "own", "sb"]}
        g8 = P.sb("g_top8", [128, 8], F32)
        g_res = Res()

    load_head_weights(0)
    sT_i = 0
    pT_i = 0
    for h in range(NH):
        wi = h % 2
        if h + 1 < NH:
            load_head_weights(h + 1)
        for s in range(8):
            sl, sl_res = slabs.load(s)
            for dc in range(8):
                P.op("pe", lambda e, dc=dc, sl=sl, wi=wi: e.matmul(bank[:, 0, :], wh[wi][:, dc, 1, :], sl[:, dc, :],
                                                                  start=(dc == 0), stop=(dc == 7)),
                     reads=[sl_res, wh_res[wi]], writes=[bank_res[0]])
            P.op("act", lambda e, s=s: e.activation(out=kT[:, s * 512:(s + 1) * 512], in_=bank[:, 0, :], func=AF.Copy),
                 reads=[bank_res[0]], writes=[kT_res[s]])
            for tt in range(4):
                for dc in range(8):
                    P.op("pe", lambda e, dc=dc, sl=sl, wi=wi, tt=tt: e.matmul(
                        bank[:, 1, tt * 128:(tt + 1) * 128], sl[:, dc, tt * 128:(tt + 1) * 128], wh[wi][:, dc, 2, :],
                        start=(dc == 0), stop=(dc == 7)),
                        reads=[sl_res, wh_res[wi]], writes=[bank_res[1]])
            P.op("act", lambda e, s=s: e.activation(out=vv[:, s * 4:(s + 1) * 4, :],
                                                    in_=bank[:, 1, :].rearrange("p (t c) -> p t c", t=4), func=AF.Copy),
                 reads=[bank_res[1]], writes=[vv_res[s]])
            if s < 4:
                for dc in range(8):
                    P.op("pe", lambda e, dc=dc, sl=sl, wi=wi: e.matmul(bank[:, 0, :], wh[wi][:, dc, 0, :], sl[:, dc, :],
                                                                      start=(dc == 0), stop=(dc == 7)),
                         reads=[sl_res, wh_res[wi]], writes=[bank_res[0]])
                P.op("act", lambda e, s=s: e.activation(out=qT[:, s * 512:(s + 1) * 512], in_=bank[:, 0, :], func=AF.Copy,
                                                        scale=scale),
                     reads=[bank_res[0]], writes=[qT_res[s]])
        if kind == "moba":
            P.op("dve", lambda e: e.tensor_reduce(out=kms[:], in_=kT[:].rearrange("p (n k) -> p n k", k=256), axis=AX.X,
                                                  op=ALU.add),
                 reads=kT_res, writes=[km_res])
            P.op("dve", lambda e: e.tensor_scalar(out=kmT[:], in0=kms[:], scalar1=1.0 / 256.0, scalar2=None, op0=ALU.mult),
                 reads=[km_res], writes=[km_res])
            for t in range(NT):
                P.op("pe", lambda e, t=t: e.matmul(bank[:, 0, 0:16], qT[:, t * 128:(t + 1) * 128], kmT[:], start=True, stop=True),
                     reads=[qT_res[t // 4], km_res], writes=[bank_res[0]])
                G_ = gsc
                P.op("dve", lambda e, t=t: e.tensor_scalar(out=G_["past"][:], in0=bposB[:], scalar1=ownb[:, t:t + 1], scalar2=None,
                                                           op0=ALU.is_lt), reads=[cres], writes=[g_res])
                P.op("dve", lambda e: e.tensor_scalar(out=G_["nb"][:], in0=G_["past"][:], scalar1=-1.0, scalar2=1e30,
                                                      op0=ALU.add, op1=ALU.mult), reads=[g_res], writes=[g_res])
                P.op("dve", lambda e: e.tensor_tensor(out=G_["gm"][:], in0=bank[:, 0, 0:16], in1=G_["past"][:], op=ALU.mult),
                     reads=[bank_res[0], g_res], writes=[g_res])
                P.op("dve", lambda e: e.tensor_tensor(out=G_["gm"][:], in0=G_["gm"][:], in1=G_["nb"][:], op=ALU.add),
                     reads=[g_res], writes=[g_res])
                P.op("dve", lambda e: e.max(out=g8[:], in_=G_["gm"][:]), reads=[g_res], writes=[g_res])
                P.op("dve", lambda e: e.tensor_scalar(out=G_["sel"][:], in0=G_["gm"][:], scalar1=g8[:, 2:3], scalar2=None,
                                                      op0=ALU.is_ge), reads=[g_res], writes=[g_res])
                P.op("dve", lambda e: e.tensor_tensor(out=G_["sel"][:], in0=G_["sel"][:], in1=G_["past"][:], op=ALU.mult),
                     reads=[g_res], writes=[g_res])
                P.op("dve", lambda e, t=t: e.tensor_scalar(out=G_["own"][:], in0=bposB[:], scalar1=ownb[:, t:t + 1], scalar2=None,
                                                           op0=ALU.is_equal), reads=[cres, g_res], writes=[g_res])
                P.op("dve", lambda e: e.tensor_tensor(out=G_["sel"][:], in0=G_["sel"][:], in1=G_["own"][:], op=ALU.max),
                     reads=[g_res], writes=[g_res])
                P.op("dve", lambda e: e.tensor_scalar(out=G_["sb"][:], in0=G_["sel"][:], scalar1=-1.0, scalar2=-NEGBIG,
                                                      op0=ALU.add, op1=ALU.mult), reads=[g_res], writes=[g_res])
                P.op("pe", lambda e, t=t: e.transpose(out=bank[0:16, 1, (t % 4) * 128:(t % 4 + 1) * 128], in_=G_["sb"][:],
                                                      identity=ident[:]),
                     reads=[g_res, cres], writes=[bank_res[1]])
                if t % 4 == 3:
                    P.op("act", lambda e, t=t: e.activation(out=selbT[:, (t - 3) * 128:(t + 1) * 128], in_=bank[0:16, 1, :],
                                                            func=AF.Copy),
                         reads=[bank_res[1]], writes=[selb_res])
        for ci in range(4):
            ob = 4 + (ci % 2)
            sb_ = 6 + (ci % 2)
            kcs = key_chunks(ci)
            maskT, mask_res = gen_masks(P, C, ci)
            ai = ci % 2
            nk = len(kcs)
            tiles = []
            for n, (jl, masked) in enumerate(kcs):
                tiles.append(dict(n=n, jl=jl, masked=masked, sbk=2 + (sT_i % 2), pi=pT_i % 3))
                sT_i += 1
                pT_i += 1

            def stage1(T, ci=ci, h=h, maskT=maskT, mask_res=mask_res):
                jl, masked, sbk, pi = T["jl"], T["masked"], T["sbk"], T["pi"]
                P.op("pe", lambda e: e.matmul(
                    bank[:, sbk, :], kT[:, jl * 128:(jl + 1) * 128], qT[:, ci * 512:(ci + 1) * 512], start=True, stop=False),
                    reads=[kT_res[jl // 4], qT_res[ci]], writes=[bank_res[sbk]])
                if kind == "fox":
                    P.op("pe", lambda e: e.matmul(
                        bank[:, sbk, :], selh[:, h, :], c3[:, ci * 512:(ci + 1) * 512], start=False, stop=(not masked)),
                        reads=[fox_res, cres], writes=[bank_res[sbk]])
                else:
                    P.op("pe", lambda e: e.matmul(
                        bank[:, sbk, :], esel[:, jl // 2, :], selbT[:, ci * 512:(ci + 1) * 512], start=False, stop=(not masked)),
                        reads=[selb_res, cres], writes=[bank_res[sbk]])
                if masked:
                    mi = mask_index(ci, jl) % 8
                    P.op("pe", lambda e: e.matmul(bank[:, sbk, :], identb[:], maskT[:, mi, :], start=False, stop=True),
                         reads=[mask_res, cres], writes=[bank_res[sbk]])
                if kind == "fox":
                    P.op("act", lambda e: e.activation(
                        out=pT[pi][:], in_=bank[:, sbk, :], func=AF.Exp, bias=Ck[:, jl, h:h + 1], scale=1.0),
                        reads=[bank_res[sbk], fox_res], writes=[pT_res[pi]])
                else:
                    P.op("act", lambda e: e.activation(out=pT[pi][:], in_=bank[:, sbk, :], func=AF.Exp),
                         reads=[bank_res[sbk]], writes=[pT_res[pi]])

            def stage2(T, ob=ob, ai=ai, nk=nk):
                n, jl, pi = T["n"], T["jl"], T["pi"]
                P.op("pe", lambda e: e.matmul(
                    bank[:, ob, :], vv[:, jl, :], pT[pi][:], start=(n == 0), stop=(n == nk - 1)),
                    reads=[pT_res[pi], vv_res[jl // 4]], writes=[bank_res[ob]])
                if n == 0:
                    P.op("dve", lambda e: e.tensor_copy(out=pacc[ai][:], in_=pT[pi][:]),
                         reads=[pT_res[pi]], writes=[pacc_res[ai]])
                else:
                    P.op("dve", lambda e: e.tensor_tensor(out=pacc[ai][:], in0=pacc[ai][:], in1=pT[pi][:], op=ALU.add),
                         reads=[pT_res[pi], pacc_res[ai]], writes=[pacc_res[ai]])

            stage1(tiles[0])
            for n in range(nk):
                if n + 1 < nk:
                    stage1(tiles[n + 1])
                stage2(tiles[n])
            P.op("pe", lambda e, sb_=sb_, ai=ai: e.matmul(bank[:, sb_, :], onesf[:], pacc[ai][:], start=True, stop=True),
                 reads=[pacc_res[ai], cres], writes=[bank_res[sb_]])
            P.op("dve", lambda e, sb_=sb_: e.reciprocal(out=rs[:], in_=bank[:, sb_, :]), reads=[bank_res[sb_]], writes=[rs_res])
            P.op("dve", lambda e, ob=ob, ci=ci: e.tensor_tensor(out=oT[:, ci * 512:(ci + 1) * 512], in0=bank[:, ob, :], in1=rs[:],
                                                                op=ALU.mult),
                 reads=[bank_res[ob], rs_res], writes=[oT_res[ci]])
        for t in range(NT):
            for hf in range(2):
                bk = hf
                P.op("pe", lambda e, t=t, hf=hf, bk=bk, wi=wi: e.matmul(
                    bank[:, bk, :], oT[:, t * 128:(t + 1) * 128], wo[wi][:, hf * 512:(hf + 1) * 512], start=True, stop=True),
                    reads=[oT_res[t // 4], wo_res[wi]], writes=[bank_res[bk]])
                P.op("dve", lambda e, t=t, hf=hf, bk=bk: e.tensor_tensor(
                    out=xm[:, t, hf * 512:(hf + 1) * 512], in0=bank[:, bk, :], in1=xm[:, t, hf * 512:(hf + 1) * 512], op=ALU.add),
                    reads=[bank_res[bk], xm_res[t]], writes=[xm_res[t]])


def emit_retention_mixer(P, C, dr):
    bank, bank_res = C["bank"], C["bank_res"]
    cres = C["const_res"]
    xm, xm_res = C["xm"], C["xm_res"]
    qposB, kposc, ident = C["qposB"], C["kposc"], C["ident"]
    w_in, w_out = dr["w_in"], dr["w_out"]
    NH, DK, DV = 4, 256, 512
    slabs = SlabLoader(P, dr)
    wA = P.sb("wA", [128, 8, 512], BF16)
    wV = P.sb("wV", [128, 8, 512], BF16)
    wG = P.sb("wG", [128, 8, 512], BF16)
    wo = P.sb("wo", [128, 4, D], BF16)
    gng = P.sb("gng", [128, 512], F32)
    wA_res, wV_res, wG_res, wo_res, gng_res = Res(), Res(), Res(), Res(), Res()
    kT = P.sb("kT", [128, 2, S], BF16)
    kT_res = [Res() for _ in range(8)]
    vv = P.sb("vv", [128, 32, 512], BF16)
    vv_res = [Res() for _ in range(8)]
    qT = P.sb("qT", [128, 2, 512], BF16)
    qT_res = Res()
    gs = P.sb("gs", [128, 4, 512], BF16)
    gs_res = [Res() for _ in range(4)]
    oT = P.sb("oT", [128, 4, 512], BF16)
    oT_res = [Res() for _ in range(4)]
    ts = [P.sb(f"ts{i}", [128, 512], F32) for i in range(2)]
    ts_res = [Res(), Res()]
    cs = P.sb("cs", [128, 2, 512], F32)
    cs_res = Res()
    ta = P.sb("ta", [128, 512], F32)
    tb2 = P.sb("tb2", [128, 512], F32)
    ta_res, tb_res = Res(), Res()
    dm, dm_res = ts, ts_res
    pen, pen_res = ta, ta_res
    Dt = [tb2, P.sb("Dt1", [128, 512], F32)]
    Dt_res = [tb_res, Res()]
    pT = [P.sb(f"pT{i}", [128, 512], BF16) for i in range(3)]
    pT_res = [Res() for _ in range(3)]
    on = [P.sb(f"on{i}", [128, 512], F32) for i in range(2)]
    on_res = [Res(), Res()]
    gst = P.sb("gst", [128, 6], F32)
    gmv = P.sb("gmv", [128, 2], F32)
    gsd = P.sb("gsd", [128, 4], F32)
    gn_res = Res()

    def load_head_weights(h):
        def ld(dst, dres, c0, n, off=0):
            src = w_in[:, c0:c0 + n].rearrange("(dc p) c -> p dc c", p=128)
            P.dma("pool", lambda e: e.dma_start(out=dst[:, :, off:off + n], in_=src), writes=[dres])
        ld(wA, wA_res, h * DK, DK, 0)
        ld(wA, wA_res, D + h * DK, DK, DK)
        ld(wV, wV_res, 2 * D + h * DV, DV)
        ld(wG, wG_res, 4 * D + h * DV, DV)
        src = w_out[h * DV:(h + 1) * DV, :].rearrange("(c p) n -> p c n", p=128)
        P.dma("pool", lambda e: e.dma_start(out=wo[:], in_=src), writes=[wo_res])
        P.dma("sp", lambda e: e.dma_start(out=gng[:], in_=dr["gngB"][:, h * DV:(h + 1) * DV]), writes=[gng_res])

    def project_rot(sl, sl_res, s, col0, scale, dst, dst_res):
        for c in range(2):
            for dc in range(8):
                P.op("pe", lambda e, c=c, dc=dc: e.matmul(bank[:, c, :], wA[:, dc, col0 + c * 128:col0 + (c + 1) * 128], sl[:, dc, :],
                                                          start=(dc == 0), stop=(dc == 7)),
                     reads=[sl_res, wA_res], writes=[bank_res[c]])
            P.op("act", lambda e, c=c: e.activation(out=ts[c][:], in_=bank[:, c, :], func=AF.Copy, scale=scale),
                 reads=[bank_res[c]], writes=[ts_res[c]])
        P.dma("sp", lambda e: e.dma_start(out=cs[:, 0, :], in_=dr["cosT"][:, s * 512:(s + 1) * 512]), writes=[cs_res])
        P.dma("sp", lambda e: e.dma_start(out=cs[:, 1, :], in_=dr["sinT"][:, s * 512:(s + 1) * 512]), writes=[cs_res])
        P.op("dve", lambda e: e.tensor_tensor(out=ta[:], in0=ts[0][:], in1=cs[:, 0, :], op=ALU.mult),
             reads=[ts_res[0], cs_res], writes=[ta_res])
        P.op("pool", lambda e: e.tensor_tensor(out=tb2[:], in0=ts[1][:], in1=cs[:, 1, :], op=ALU.mult),
             reads=[ts_res[1], cs_res], writes=[tb_res])
        P.op("dve", lambda e: e.tensor_tensor(out=dst[0], in0=ta[:], in1=tb2[:], op=ALU.subtract),
             reads=[ta_res, tb_res], writes=[dst_res])
        P.op("dve", lambda e: e.tensor_tensor(out=ta[:], in0=ts[0][:], in1=cs[:, 1, :], op=ALU.mult),
             reads=[ts_res[0], cs_res], writes=[ta_res])
        P.op("dve", lambda e: e.tensor_tensor(out=tb2[:], in0=ts[1][:], in1=cs[:, 0, :], op=ALU.mult),
             reads=[ts_res[1], cs_res], writes=[tb_res])
        P.op("pool", lambda e: e.tensor_tensor(out=dst[1], in0=ta[:], in1=tb2[:], op=ALU.add),
             reads=[ta_res, tb_res], writes=[dst_res])

    sT_i = pT_i = dm_i = on_i = 0
    for h in range(NH):
        lg = float(np.log(1.0 - 2.0 ** (-5.0 - h)))
        load_head_weights(h)
        for s in range(8):
            sl, sl_res = slabs.load(s)
            project_rot(sl, sl_res, s, DK, DK ** -0.5,
                        (kT[:, 0, s * 512:(s + 1) * 512], kT[:, 1, s * 512:(s + 1) * 512]), kT_res[s])
            for tt in range(4):
                bk = tt % 2
                for dc in range(8):
                    P.op("pe", lambda e, dc=dc, tt=tt, bk=bk, sl=sl: e.matmul(bank[:, bk, :], sl[:, dc, tt * 128:(tt + 1) * 128], wV[:, dc, :],
                                                                            start=(dc == 0), stop=(dc == 7)),
                         reads=[sl_res, wV_res], writes=[bank_res[bk]])
                P.op("act", lambda e, s=s, tt=tt, bk=bk: e.activation(out=vv[:, s * 4 + tt, :], in_=bank[:, bk, :], func=AF.Copy),
                     reads=[bank_res[bk]], writes=[vv_res[s]])
        for ci in range(4):
            sl, sl_res = slabs.load(ci)
            project_rot(sl, sl_res, ci, 0, 1.0, (qT[:, 0, :], qT[:, 1, :]), qT_res)
            kcs = key_chunks(ci)
            nk = len(kcs)
            tiles = []
            for n, (jl, masked) in enumerate(kcs):
                tiles.append(dict(n=n, jl=jl, masked=masked, sbk=2 + (sT_i % 2), di=dm_i % 2, pi=pT_i % 3))
                sT_i += 1
                dm_i += 1
                pT_i += 1

            def stage1(T, ci=ci, lg=lg):
                jl, masked, sbk, di, pi = T["jl"], T["masked"], T["sbk"], T["di"], T["pi"]
                for c in range(2):
                    P.op("pe", lambda e, c=c: e.matmul(bank[:, sbk, :], kT[:, c, jl * 128:(jl + 1) * 128], qT[:, c, :],
                                                       start=(c == 0), stop=(c == 1)),
                         reads=[kT_res[jl // 4], qT_res], writes=[bank_res[sbk]])
                P.op("dve", lambda e: e.tensor_scalar(
                    out=dm[di][:], in0=qposB[:, ci * 512:(ci + 1) * 512], scalar1=kposc[:, jl:jl + 1], scalar2=None,
                    op0=ALU.subtract), reads=[cres], writes=[dm_res[di]])
                if masked:
                    P.op("dve", lambda e: e.tensor_scalar(out=pen[:], in0=dm[di][:], scalar1=0.0, scalar2=1.0e6,
                                                          op0=ALU.is_lt, op1=ALU.mult),
                         reads=[dm_res[di]], writes=[pen_res])
                    P.op("dve", lambda e: e.tensor_tensor(out=dm[di][:], in0=dm[di][:], in1=pen[:], op=ALU.add),
                         reads=[dm_res[di], pen_res], writes=[dm_res[di]])
                P.op("act", lambda e: e.activation(out=Dt[di][:], in_=dm[di][:], func=AF.Exp, scale=lg),
                     reads=[dm_res[di]], writes=[Dt_res[di]])
                P.op("dve", lambda e: e.tensor_tensor(out=pT[pi][:], in0=bank[:, sbk, :], in1=Dt[di][:], op=ALU.mult),
                     reads=[bank_res[sbk], Dt_res[di]], writes=[pT_res[pi]])

            def stage2(T, nk=nk):
                n, jl, pi = T["n"], T["jl"], T["pi"]
                for sub in range(4):
                    P.op("pe", lambda e, sub=sub: e.matmul(
                        bank[:, 4 + sub, :], pT[pi][:, sub * 128:(sub + 1) * 128], vv[:, jl, :], start=(n == 0), stop=(n == nk - 1)),
                        reads=[pT_res[pi], vv_res[jl // 4]], writes=[bank_res[4 + sub]])

            stage1(tiles[0])
            for n in range(nk):
                if n + 1 < nk:
                    stage1(tiles[n + 1])
                stage2(tiles[n])
            for tt in range(4):
                bk = tt % 2
                for dc in range(8):
                    P.op("pe", lambda e, dc=dc, tt=tt, bk=bk, sl=sl: e.matmul(bank[:, bk, :], sl[:, dc, tt * 128:(tt + 1) * 128], wG[:, dc, :],
                                                                            start=(dc == 0), stop=(dc == 7)),
                         reads=[sl_res, wG_res], writes=[bank_res[bk]])
                P.op("act", lambda e, tt=tt, bk=bk: e.activation(out=gs[:, tt, :], in_=bank[:, bk, :], func=AF.Silu),
                     reads=[bank_res[bk]], writes=[gs_res[tt]])
            for sub in range(4):
                ob = 4 + sub
                oi = on_i % 2
                on_i += 1
                P.op("dve", lambda e, ob=ob: e.bn_stats(out=gst[:, 0:6], in_=bank[:, ob, :]), reads=[bank_res[ob]], writes=[gn_res])
                P.op("dve", lambda e: e.bn_aggr(out=gmv[:, 0:2], in_=gst[:, 0:6]), reads=[gn_res], writes=[gn_res])
                P.op("dve", lambda e: e.tensor_scalar(out=gsd[:, 0:1], in0=gmv[:, 1:2], scalar1=1e-6, scalar2=None, op0=ALU.add),
                     reads=[gn_res], writes=[gn_res])
                P.op("act", lambda e: e.activation(out=gsd[:, 1:2], in_=gsd[:, 0:1], func=AF.Sqrt), reads=[gn_res], writes=[gn_res])
                P.op("dve", lambda e: e.reciprocal(out=gsd[:, 2:3], in_=gsd[:, 1:2]), reads=[gn_res], writes=[gn_res])
                P.op("dve", lambda e, ob=ob, oi=oi: e.tensor_scalar(out=on[oi][:], in0=bank[:, ob, :], scalar1=gmv[:, 0:1], scalar2=gsd[:, 2:3],
                                                                   op0=ALU.subtract, op1=ALU.mult),
                     reads=[bank_res[ob], gn_res], writes=[on_res[oi]])
                P.op("pool", lambda e, oi=oi: e.tensor_tensor(out=on[oi][:], in0=on[oi][:], in1=gng[:], op=ALU.mult),
                     reads=[on_res[oi], gng_res], writes=[on_res[oi]])
                P.op("pool", lambda e, oi=oi, sub=sub: e.tensor_tensor(out=on[oi][:], in0=on[oi][:], in1=gs[:, sub, :], op=ALU.mult),
                     reads=[on_res[oi], gs_res[sub]], writes=[on_res[oi]])
                bk = sub % 2
                for c in range(4):
                    P.op("pe", lambda e, c=c, bk=bk, oi=oi: e.transpose(out=bank[:, bk, c * 128:(c + 1) * 128], in_=on[oi][:, c * 128:(c + 1) * 128],
                                                                       identity=ident[:]),
                         reads=[on_res[oi], cres], writes=[bank_res[bk]])
                P.op("act", lambda e, bk=bk, sub=sub: e.activation(out=oT[:, :, sub * 128:(sub + 1) * 128],
                                                                   in_=bank[:, bk, :].rearrange("p (c q) -> p c q", c=4), func=AF.Copy),
                     reads=[bank_res[bk]], writes=[oT_res[sub]])
            for sub in range(4):
                t = ci * 4 + sub
                for hf in range(2):
                    bk = hf
                    for c in range(4):
                        P.op("pe", lambda e, c=c, sub=sub, hf=hf, bk=bk: e.matmul(
                            bank[:, bk, :], oT[:, c, sub * 128:(sub + 1) * 128], wo[:, c, hf * 512:(hf + 1) * 512],
                            start=(c == 0), stop=(c == 3)),
                            reads=[oT_res[sub], wo_res], writes=[bank_res[bk]])
                    P.op("dve", lambda e, t=t, hf=hf, bk=bk: e.tensor_tensor(
                        out=xm[:, t, hf * 512:(hf + 1) * 512], in0=bank[:, bk, :], in1=xm[:, t, hf * 512:(hf + 1) * 512], op=ALU.add),
                        reads=[bank_res[bk], xm_res[t]], writes=[xm_res[t]])


def emit_mixer(P, C, dr, kind):
    P.push_scope()
    emit_mixer_common(P, C, dr)
    if kind in ("fox", "moba"):
        emit_masks(P, C)
        emit_attention_mixer(P, C, dr, kind)
    else:
        emit_retention_mixer(P, C, dr)
    P.pop_scope()
    P.push_scope()
    lnp1 = P.sb("lnp1", [128, 2, D], F32)
    P.dma("sp", lambda e: e.dma_start(out=lnp1[:], in_=dr["lnp"][:, 0:2, :]), writes=[C["const_res"]])
    xm, xm_res = C["xm"], C["xm_res"]
    for t in range(NT):
        emit_layernorm(P, C, xm[:, t, :], xm_res[t], xm[:, t, :], xm_res[t], lnp1[:, 0, :], lnp1[:, 1, :])
    P.pop_scope()


def load_const(P, C, name, dram_ap, shape, dtype=F32, queue="sp"):
    t = P.sb(name, shape, dtype)
    P.dma(queue, lambda e: e.dma_start(out=t[:], in_=dram_ap), reads=[], writes=[C["const_res"]])
    C[name] = t
    return t


def build_program(kind, ne_run=NE, stage=9, mixer_only=False):
    nc = bass.Bass("TRN2", target_bir_lowering=False)
    P = Prog(nc)
    C = {}
    dr = {}

    def din(name, shape):
        dr[name] = nc.dram_tensor(name, list(shape), F32, kind="ExternalInput").ap()
        return dr[name]

    dr["xout"] = nc.dram_tensor("xout", [NOWN, D], F32, kind="ExternalOutput").ap()
    din("ident", [128, 128])
    din("lnp", [128, 4, D])
    din("rw", [D, NE])
    din("rbB", [128, NE])
    din("w1", [ne_run, D, 2 * D])
    din("b1T", [128, NE, 16])
    din("w2", [ne_run, D, D])
    din("b2s", [NE, D])
    din("Umat", [128, 128])
    din("iota", [128, 512])
    din("jp", [128, NT, 2])
    if kind == "moe":
        din("xmid", [NOWN, D])
    else:
        din("xown", [NOWN, D])
        din("xT", [D, S])
        din("qposB", [128, NOWN])
        din("kposc", [128, 32])
    if kind == "fox":
        din("w_in", [D, 3 * D + 8])
        din("w_out", [D, D])
        din("b_f", [8, 1])
        din("Lc", [8, 64])
        din("selh", [24, 8, 128])
    elif kind == "moba":
        din("w_in", [D, 3 * D])
        din("w_out", [D, D])
        din("esel", [16, 16, 128])
        din("bposB", [128, 16])
        din("ownb", [128, 16])
    elif kind == "ret":
        din("w_in", [D, 6 * D])
        din("w_out", [2 * D, D])
        din("gngB", [128, 2 * D])
        din("cosT", [128, S])
        din("sinT", [128, S])

    C["const_res"] = Res("const")
    C["bank"] = P.ps("bank", [128, 8, 512], F32)
    C["bank_res"] = [Res(f"bank{i}") for i in range(8)]
    C["ln_st"] = [P.sb(f"ln_st{i}", [128, 12], F32) for i in range(2)]
    C["ln_mv"] = [P.sb(f"ln_mv{i}", [128, 2], F32) for i in range(2)]
    C["ln_sd"] = [P.sb(f"ln_sd{i}", [128, 4], F32) for i in range(2)]
    C["ln_res"] = [Res("ln0"), Res("ln1")]
    load_const(P, C, "ident", dr["ident"], [128, 128])
    C["xm"] = P.sb("xm", [128, NT, D], F32)
    C["xm_res"] = [Res(f"xm{t}") for t in range(NT)]

    if kind == "moe":
        for t in range(NT):
            P.dma("sp", lambda e, t=t: e.dma_start(out=C["xm"][:, t, :], in_=dr["xmid"][t * 128:(t + 1) * 128, :]),
                  reads=[], writes=[C["xm_res"][t]])
    else:
        emit_mixer(P, C, dr, kind)
    if mixer_only:
        for tt in range(NT):
            P.dma("sp", lambda e, tt=tt: e.dma_start(out=dr["xout"][tt * 128:(tt + 1) * 128, :], in_=C["xm"][:, tt, :]),
                  reads=[C["xm_res"][tt]], writes=[])
    else:
        emit_moe(P, C, dr, ne_run, stage)
    P.finish()
    return nc


KINDS = ["fox", "moba", "ret", "fox"]
PAIR_ORDER = np.concatenate([np.arange(c * 512, (c + 1) * 512) for c in OWN_CHUNKS[0] + OWN_CHUNKS[1]])


def build_fused(n_layers=4, ne_run=NE):
    nc = bass.Bass("TRN2", target_bir_lowering=False)
    P = Prog(nc)
    C = {}
    E = {}

    def din(name, shape):
        E[name] = nc.dram_tensor(name, list(shape), F32, kind="ExternalInput").ap()
        return E[name]

    for z in range(2):
        din(f"xown{z}", [NOWN, D])
        din(f"qposB{z}", [128, NOWN])
        din(f"kposc{z}", [128, 32])
        din(f"Lc{z}", [8, 64])
        din(f"bposB{z}", [128, 16])
        din(f"ownb{z}", [128, 16])
        din(f"cosT{z}", [128, S])
        din(f"sinT{z}", [128, S])
    din("xT0", [D, S])
    din("ident", [128, 128])
    din("selh", [24, 8, 128])
    din("esel", [16, 16, 128])
    din("fox_w_in", [2, D, 3 * D + 8])
    din("fox_b_f", [2, 8, 1])
    din("fox_w_out", [2, D, D])
    din("moba_w_in", [1, D, 3 * D])
    din("moba_w_out", [1, D, D])
    din("ret_w_in", [1, D, 6 * D])
    din("ret_w_out", [1, 2 * D, D])
    din("gngB", [128, 2 * D])
    din("lnp", [4, 128, 4, D])
    din("rw", [4, D, NE])
    din("rbB", [4, 128, NE])
    din("w1", [4, ne_run, D, 2 * D])
    din("b1T", [4, 128, NE, 16])
    din("w2", [4, ne_run, D, D])
    din("b2s", [4, NE, D])
    din("Umat", [128, 128])
    din("iota", [128, 512])
    din("jp", [128, NT, 2])
    xout = nc.dram_tensor("xout", [S, D], F32, kind="ExternalOutput").ap()
    xs = {}
    xTs = {}
    for L in range(1, n_layers):
        for z in range(2):
            xs[(L, z)] = (nc.dram_tensor(f"xs{L}_{z}", [NOWN, D], F32, kind="Internal").ap(), Res())
        xTs[L] = (nc.dram_tensor(f"xTs{L}", [D, S], BF16, kind="Internal").ap(), [Res(), Res()])

    C["const_res"] = Res("const")
    C["bank"] = P.ps("bank", [128, 8, 512], F32)
    C["bank_res"] = [Res(f"bank{i}") for i in range(8)]
    C["ln_st"] = [P.sb(f"ln_st{i}", [128, 12], F32) for i in range(2)]
    C["ln_mv"] = [P.sb(f"ln_mv{i}", [128, 2], F32) for i in range(2)]
    C["ln_sd"] = [P.sb(f"ln_sd{i}", [128, 4], F32) for i in range(2)]
    C["ln_res"] = [Res("ln0"), Res("ln1")]
    load_const(P, C, "ident", E["ident"], [128, 128])

    for L in range(n_layers):
        kind = KINDS[L]
        j = L // 3
        for z in range(2):
            dr = {"qposB": E[f"qposB{z}"], "kposc": E[f"kposc{z}"], "Lc": E[f"Lc{z}"], "bposB": E[f"bposB{z}"],
                  "ownb": E[f"ownb{z}"], "cosT": E[f"cosT{z}"], "sinT": E[f"sinT{z}"], "selh": E["selh"], "esel": E["esel"],
                  "gngB": E["gngB"], "lnp": E["lnp"][L], "rw": E["rw"][L], "rbB": E["rbB"][L], "w1": E["w1"][L],
                  "b1T": E["b1T"][L], "w2": E["w2"][L], "b2s": E["b2s"][L],
                  "Umat": E["Umat"], "iota": E["iota"], "jp": E["jp"],
                  "slab_map": [z * 4 + s for s in range(4)] + [(1 - z) * 4 + s for s in range(4)]}
            if kind == "fox":
                dr.update(w_in=E["fox_w_in"][j], w_out=E["fox_w_out"][j], b_f=E["fox_b_f"][j])
            elif kind == "moba":
                dr.update(w_in=E["moba_w_in"][j], w_out=E["moba_w_out"][j])
            else:
                dr.update(w_in=E["ret_w_in"][j], w_out=E["ret_w_out"][j])
            if L == 0:
                dr.update(xown=E[f"xown{z}"], xT=E["xT0"])
            else:
                dr.update(xown=xs[(L, z)][0], xown_res=xs[(L, z)][1], xT=xTs[L][0], xT_res=xTs[L][1])
            if L == n_layers - 1:
                dr.update(xout=xout[z * NOWN:(z + 1) * NOWN, :])
            else:
                dr.update(xout=xs[(L + 1, z)][0], xout_res=xs[(L + 1, z)][1],
                          xTnext=xTs[L + 1][0], xTnext_col0=z * NOWN, xTnext_res=xTs[L + 1][1][z])
            P.push_scope()
            C["xm"] = P.sb("xm", [128, NT, D], F32)
            C["xm_res"] = [Res(f"xm{t}") for t in range(NT)]
            emit_mixer(P, C, dr, kind)
            emit_moe(P, C, dr, ne_run)
            P.pop_scope()
    P.finish()
    return nc


def fused_inputs(inputs, ne_run=NE):
    m = {"ident": np.eye(128, dtype=np.float32)}
    m.update(moe_consts())
    for z in range(2):
        t = mixer_inputs("fox", 0, z, None, inputs, tables_only=True)
        t.update(mixer_inputs("moba", 0, z, None, inputs, tables_only=True))
        t.update(mixer_inputs("ret", 0, z, None, inputs, tables_only=True))
        for k in ["qposB", "kposc", "Lc", "bposB", "ownb", "cosT", "sinT"]:
            m[f"{k}{z}"] = t[k]
        m["selh"], m["esel"] = t["selh"], t["esel"]
    m["fox_w_in"] = np.ascontiguousarray(inputs["fox_w_in"])
    m["fox_b_f"] = np.ascontiguousarray(inputs["fox_b_f"].reshape(2, 8, 1))
    m["fox_w_out"] = np.ascontiguousarray(inputs["fox_w_out"])
    m["moba_w_in"] = np.ascontiguousarray(inputs["moba_w_in"])
    m["moba_w_out"] = np.ascontiguousarray(inputs["moba_w_out"])
    m["ret_w_in"] = np.ascontiguousarray(inputs["ret_w_in"])
    m["ret_w_out"] = np.ascontiguousarray(inputs["ret_w_out"])
    m["gngB"] = np.ascontiguousarray(np.broadcast_to(inputs["ret_gn_g"][0][None], (128, 2 * D)))
    lnp = np.stack([inputs["ln_g"][:, 0], inputs["ln_b"][:, 0], inputs["ln_g"][:, 1], inputs["ln_b"][:, 1]], 1)
    m["lnp"] = np.ascontiguousarray(np.broadcast_to(lnp[:, None], (4, 128, 4, D)))
    m["rw"] = np.ascontiguousarray(inputs["router_w"])
    m["rbB"] = np.ascontiguousarray(np.broadcast_to(inputs["router_b"][:, None, :], (4, 128, NE)))
    m["w1"] = np.ascontiguousarray(inputs["moe_w1"][:, :ne_run])
    m["b1T"] = np.ascontiguousarray(inputs["moe_b1"].reshape(4, NE, 16, 128).transpose(0, 3, 1, 2))
    m["w2"] = np.ascontiguousarray(inputs["moe_w2"][:, :ne_run])
    m["b2s"] = np.ascontiguousarray(inputs["moe_b2"])
    return {k: np.asarray(v, dtype=np.float32) for k, v in m.items()}


_FUSED = {}


def run_fused(inputs, n_layers=4, cores=NB, ne_run=NE):
    key = (n_layers, ne_run)
    if key not in _FUSED:
        _FUSED[key] = build_fused(n_layers, ne_run)
    nc = _FUSED[key]
    shared = fused_inputs(inputs, ne_run)
    x = inputs["x"]
    in_maps = []
    for b in range(cores):
        m = dict(shared)
        for z in range(2):
            m[f"xown{z}"] = np.ascontiguousarray(x[b][own_index(z)])
        m["xT0"] = np.ascontiguousarray(x[b][PAIR_ORDER].T)
        in_maps.append(m)
    res = run_bass_kernel_spmd(nc, in_maps, core_ids=list(range(cores)))
    out = np.zeros((cores, S, D), np.float32)
    for b in range(cores):
        out[b][PAIR_ORDER] = res.results[b]["xout"]
    return out


def kernel(**inputs):
    inputs = {k: np.asarray(v) for k, v in inputs.items()}
    return run_fused(inputs, 4, NB, NE)


def moe_consts():
    k = np.arange(128)
    jp = np.zeros((128, NT, 2), np.float32)
    jp[:, :, 0] = (np.arange(NT) % 4)[None, :]
    jp[:, :, 1] = k[:, None]
    return {"Umat": (k[:, None] < k[None, :]).astype(np.float32),
            "iota": np.ascontiguousarray(np.broadcast_to(np.arange(512, dtype=np.float32)[None], (128, 512))),
            "jp": jp}


def moe_inputs(i, inputs):
    b1 = inputs["moe_b1"][i]
    b1T = np.ascontiguousarray(b1.reshape(NE, 16, 128).transpose(2, 0, 1))
    lnp = np.stack([inputs["ln_g"][i, 0], inputs["ln_b"][i, 0], inputs["ln_g"][i, 1], inputs["ln_b"][i, 1]], 0)
    return {
        "ident": np.eye(128, dtype=np.float32),
        "lnp": np.ascontiguousarray(np.broadcast_to(lnp[None], (128, 4, D))).astype(np.float32),
        "rw": np.ascontiguousarray(inputs["router_w"][i]),
        "rbB": np.ascontiguousarray(np.broadcast_to(inputs["router_b"][i][None], (128, NE))).astype(np.float32),
        "w1": np.ascontiguousarray(inputs["moe_w1"][i]),
        "b1T": b1T.astype(np.float32),
        "w2": np.ascontiguousarray(inputs["moe_w2"][i]),
        "b2s": np.ascontiguousarray(inputs["moe_b2"][i]),
    }


def local_pos(z):
    own = OWN_CHUNKS[z]
    peer = OWN_CHUNKS[1 - z]
    return np.concatenate([np.arange(c * 512, (c + 1) * 512) for c in own + peer])


def mixer_inputs(kind, j, z, xb, inputs, tables_only=False):
    pos = local_pos(z)
    m = {} if tables_only else {"xown": np.ascontiguousarray(xb[pos[:NOWN]]), "xT": np.ascontiguousarray(xb[pos].T)}
    m.update({
        "qposB": np.ascontiguousarray(np.broadcast_to(pos[None, :NOWN].astype(np.float32), (128, NOWN))),
        "kposc": np.ascontiguousarray(pos.reshape(32, 128).T.astype(np.float32)),
    })
    if kind == "fox":
        cpos = pos[::512] // 512
        Lc = (cpos[None, :] < cpos[:, None]).astype(np.float32)
        selh = np.zeros((24, 8, 128), np.float32)
        for h in range(8):
            selh[[h, 8 + h, 16 + h], h, :] = 1.0
        m.update({"w_in": np.ascontiguousarray(inputs["fox_w_in"][j]), "w_out": np.ascontiguousarray(inputs["fox_w_out"][j]),
                  "b_f": np.ascontiguousarray(inputs["fox_b_f"][j].reshape(8, 1)),
                  "Lc": np.ascontiguousarray(np.broadcast_to(Lc.reshape(1, 64), (8, 64))), "selh": selh})
    elif kind == "moba":
        esel = np.zeros((16, 16, 128), np.float32)
        for n in range(16):
            esel[n, n, :] = 1.0
        bpos = (pos[::256] // 256).astype(np.float32)
        m.update({"w_in": np.ascontiguousarray(inputs["moba_w_in"][j]), "w_out": np.ascontiguousarray(inputs["moba_w_out"][j]),
                  "esel": esel, "bposB": np.ascontiguousarray(np.broadcast_to(bpos[None], (128, 16))),
                  "ownb": np.ascontiguousarray((pos[:NOWN] // 256).reshape(16, 128).T.astype(np.float32))})
    else:
        inv_freq = np.exp(-np.log(10000.0) * np.arange(0, 256, 2, dtype=np.float32) / 256.0).astype(np.float32)
        ang = pos[None, :].astype(np.float32) * inv_freq[:, None]
        m.update({"w_in": np.ascontiguousarray(inputs["ret_w_in"][j]), "w_out": np.ascontiguousarray(inputs["ret_w_out"][j]),
                  "gngB": np.ascontiguousarray(np.broadcast_to(inputs["ret_gn_g"][j][None], (128, 2 * D))),
                  "cosT": np.cos(ang).astype(np.float32), "sinT": np.sin(ang).astype(np.float32)})
    return m


def own_index(z):
    return np.concatenate([np.arange(c * 512, (c + 1) * 512) for c in OWN_CHUNKS[z]])


_PROGS = {}


def get_prog(kind):
    if kind not in _PROGS:
        _PROGS[kind] = build_program(kind)
    return _PROGS[kind]


def run_moe_only(i, xmid, inputs):
    nc = get_prog("moe")
    common = moe_inputs(i, inputs)
    in_maps = []
    for c in range(8):
        b, z = c // 2, c % 2
        m = dict(common)
        m["xmid"] = np.ascontiguousarray(xmid[b][own_index(z)])
        in_maps.append(m)
    res = run_bass_kernel_spmd(nc, in_maps, core_ids=list(range(8)))
    out = np.zeros((NB, S, D), np.float32)
    for c in range(8):
        b, z = c // 2, c % 2
        out[b][own_index(z)] = res.results[c]["xout"]
    return out
```

```python
import contextlib
import numpy as np
import concourse.bass as bass
import concourse.mybir as mybir
from concourse.bass_utils import run_bass_kernel_spmd

F32 = mybir.dt.float32
BF16 = mybir.dt.bfloat16
AF = mybir.ActivationFunctionType
ALU = mybir.AluOpType
AX = mybir.AxisListType

D = 1024
S = 4096
NB = 4
NOWN = 2048
NT = 16
NE = 32
ALPHA = 8.0 ** 0.25
LN_EPS = 1e-5
OWN_CHUNKS = {0: [0, 3, 4, 7], 1: [1, 2, 5, 6]}

ENGS = ["pe", "act", "dve", "pool", "sp"]
NDMA = 12


class Res:
    __slots__ = ("name", "w", "r")

    def __init__(self, name=""):
        self.name = name
        self.w = None
        self.r = {}


class Prog:
    def __init__(self, nc):
        self.nc = nc
        self.es = contextlib.ExitStack()
        self.scopes = [self.es]
        self.q = {e: [] for e in ENGS}
        self.sem = {}
        for e in ["pe", "act", "dve", "pool"]:
            self.sem[e] = nc.alloc_semaphore("s_" + e)
        self.cnt = {e: 0 for e in ENGS}
        self.dsem = {q: [nc.alloc_semaphore(f"d_{q}{i}") for i in range(NDMA)] for q in ["sp", "pool"]}
        self.dcnt = {"sp": 0, "pool": 0}
        self.waited = {e: {} for e in ENGS}

    def sb(self, name, shape, dtype):
        self.nalloc = getattr(self, "nalloc", 0) + 1
        return self.scopes[-1].enter_context(self.nc.sbuf_tensor(f"sb{self.nalloc}_{name}", list(shape), dtype))

    def ps(self, name, shape, dtype=F32):
        return self.es.enter_context(self.nc.psum_tensor("ps_" + name, list(shape), dtype))

    def push_scope(self):
        self.scopes.append(contextlib.ExitStack())

    def pop_scope(self):
        self.barrier()
        self.scopes.pop().close()

    def _deps(self, reads, writes):
        deps = []
        for r in reads:
            if r.w is not None:
                deps.append(r.w)
        for w in writes:
            if w.w is not None:
                deps.append(w.w)
            deps.extend(w.r.values())
        return deps

    def _emit_waits(self, eng, deps):
        wd = self.waited[eng]
        need = {}
        for (sem, val) in deps:
            k = id(sem)
            if wd.get(k, 0) >= val:
                continue
            if k not in need or need[k][1] < val:
                need[k] = (sem, val)
        for k, (sem, val) in need.items():
            wd[k] = val
            self.q[eng].append(("wait", sem, val))

    def _mark(self, tok, reads, writes):
        sem, val = tok
        k = id(sem)
        for r in reads:
            if k not in r.r or r.r[k][1] < val:
                r.r[k] = tok
        for w in writes:
            w.w = tok
            w.r = {}

    def op(self, eng, fn, reads=(), writes=()):
        deps = self._deps(reads, writes)
        if eng == "pe":
            deps = [d for d in deps if d[0] is not self.sem["pe"]]
        self._emit_waits(eng, deps)
        self.cnt[eng] += 1
        tok = (self.sem[eng], self.cnt[eng])
        self.q[eng].append(("op", fn, self.sem[eng]))
        self._mark(tok, reads, writes)
        return tok

    def dma(self, queue, fn, reads=(), writes=()):
        deps = self._deps(reads, writes)
        k = self.dcnt[queue]
        self.dcnt[queue] += 1
        sem = self.dsem[queue][k % NDMA]
        prev = 16 * (k // NDMA)
        if prev > 0:
            deps.append((sem, prev))
        self._emit_waits(queue, deps)
        tok = (sem, prev + 16)
        self.q[queue].append(("dma", fn, sem))
        self._mark(tok, reads, writes)
        return tok

    def all_tokens(self):
        toks = [(self.sem[e], self.cnt[e]) for e in ["pe", "act", "dve", "pool"] if self.cnt[e] > 0]
        for q in ["sp", "pool"]:
            k = self.dcnt[q]
            for i in range(min(k, NDMA)):
                n_i = (k - 1 - i) // NDMA + 1
                toks.append((self.dsem[q][i], 16 * n_i))
        return toks

    def barrier(self):
        toks = self.all_tokens()
        for e in ENGS:
            self._emit_waits(e, toks)

    def finish(self):
        self._emit_waits("sp", self.all_tokens())
        nc = self.nc
        qs = self.q

        def run(engobj, items):
            for it in items:
                if it[0] == "wait":
                    engobj.wait_ge(it[1], it[2])
                elif it[0] == "op":
                    it[1](engobj).then_inc(it[2], 1)
                else:
                    it[1](engobj).then_inc(it[2], 16)

        with nc.Block() as block:
            @block.sync
            def _(e):
                run(e, qs["sp"])

            @block.tensor
            def _(e):
                run(e, qs["pe"])

            @block.scalar
            def _(e):
                run(e, qs["act"])

            @block.vector
            def _(e):
                run(e, qs["dve"])

            @block.gpsimd
            def _(e):
                run(e, qs["pool"])
        self.es.close()


def emit_layernorm(P, C, src, src_res, dst, dst_res, g, b, eng2="pool"):
    st, mv, sd, r = C["ln_st"], C["ln_mv"], C["ln_sd"], C["ln_res"]
    for hlf in range(2):
        P.op("dve", lambda e, hlf=hlf: e.bn_stats(out=st[:, hlf * 6:(hlf + 1) * 6],
                                                   in_=src[:, hlf * 512:(hlf + 1) * 512]),
             reads=[src_res], writes=[r])
    P.op("dve", lambda e: e.bn_aggr(out=mv[:, 0:2], in_=st[:, 0:12]), reads=[r], writes=[r])
    P.op("dve", lambda e: e.tensor_scalar(out=sd[:, 0:1], in0=mv[:, 1:2], scalar1=LN_EPS, scalar2=None,
                                          op0=ALU.add), reads=[r], writes=[r])
    P.op("act", lambda e: e.activation(out=sd[:, 1:2], in_=sd[:, 0:1], func=AF.Sqrt), reads=[r], writes=[r])
    P.op("dve", lambda e: e.reciprocal(out=sd[:, 2:3], in_=sd[:, 1:2]), reads=[r], writes=[r])
    P.op("dve", lambda e: e.tensor_scalar(out=dst, in0=src, scalar1=mv[:, 0:1], scalar2=sd[:, 2:3],
                                          op0=ALU.subtract, op1=ALU.mult),
         reads=[src_res, r], writes=[dst_res])
    P.op(eng2, lambda e: e.tensor_tensor(out=dst, in0=dst, in1=g, op=ALU.mult),
         reads=[dst_res, C["const_res"]], writes=[dst_res])
    P.op(eng2, lambda e: e.tensor_tensor(out=dst, in0=dst, in1=b, op=ALU.add),
         reads=[dst_res, C["const_res"]], writes=[dst_res])


def emit_moe(P, C, dr, ne_run=NE, stage=9):
    xm, xm_res = C["xm"], C["xm_res"]
    bank, bank_res = C["bank"], C["bank_res"]
    ident = C["ident"]
    cres = C["const_res"]
    P.push_scope()
    rw = load_const(P, C, "rw", dr["rw"].rearrange("(kc q) n -> q kc n", q=128), [128, 8, NE])
    rbB = load_const(P, C, "rbB", dr["rbB"], [128, NE])
    b1T = load_const(P, C, "b1T", dr["b1T"], [128, NE, 16])
    b2s = load_const(P, C, "b2s", dr["b2s"], [NE, D])
    Um = load_const(P, C, "Um", dr["Umat"], [128, 128])
    iota = load_const(P, C, "iota", dr["iota"], [128, 512])
    jp = load_const(P, C, "jp", dr["jp"], [128, NT, 2])
    onesf = P.sb("onesf", [128, 128], F32)
    P.op("dve", lambda e: e.memset(onesf[:], 1.0), writes=[cres])
    xb = P.sb("xb", [128, NT, D], BF16)
    xb_res = [Res() for _ in range(NT)]
    xTf = P.sb("xTf", [128, 8, 128], F32)
    xTf_res = [Res(), Res()]
    G = P.sb("G", [128, NT, NE], F32)
    M = P.sb("M", [128, NT, NE], F32)
    R = P.sb("R", [128, NT, NE], F32)
    G_res = [Res() for _ in range(NT)]
    R_res = [Res() for _ in range(NT)]
    GTt = P.sb("GTt", [NE, 128], F32)
    GTt_res = Res()
    cm = P.sb("cm", [128, NE], F32)
    cm_res = Res()
    TB = P.sb("TB", [128, NT, 4], BF16)
    TBf = P.sb("TBf", [128, NT], F32)
    TB_res = Res()
    lg = P.sb("lg", [128, NE], F32)
    top8 = P.sb("top8", [128, 8], F32)
    rt = P.sb("rt", [128, 4], F32)
    ex = P.sb("ex", [128, NE], F32)
    r_res = Res()
    P.op("dve", lambda e: e.tensor_copy(out=TB[:, :, 0:2], in_=jp[:]), reads=[cres], writes=[TB_res])

    def route_a(t):
        lb = 2 if t % 2 == 0 else 7
        P.op("pool", lambda e, t=t: e.tensor_copy(out=xb[:, t, :], in_=xm[:, t, :]), reads=[xm_res[t]], writes=[xb_res[t]])
        for hb in range(2):
            bk = hb
            for j in range(4):
                dc = hb * 4 + j
                P.op("pe", lambda e, t=t, dc=dc, j=j, bk=bk: e.transpose(
                    out=bank[:, bk, j * 128:(j + 1) * 128], in_=xm[:, t, dc * 128:(dc + 1) * 128], identity=ident[:]),
                    reads=[xm_res[t], cres], writes=[bank_res[bk]])
            P.op("act", lambda e, hb=hb, bk=bk: e.activation(
                out=xTf[:, hb * 4:(hb + 1) * 4, :], in_=bank[:, bk, :].rearrange("p (j c) -> p j c", j=4),
                func=AF.Copy), reads=[bank_res[bk]], writes=[xTf_res[hb]])
        for dc in range(8):
            P.op("pe", lambda e, dc=dc, lb=lb: e.matmul(bank[:, lb, 0:NE], xTf[:, dc, :], rw[:, dc, :],
                                                       start=(dc == 0), stop=(dc == 7)),
                 reads=[xTf_res[dc // 4], cres], writes=[bank_res[lb]])

    def route_b(t):
        lb = 2 if t % 2 == 0 else 7
        P.op("dve", lambda e, lb=lb: e.tensor_tensor(out=lg[:], in0=bank[:, lb, 0:NE], in1=rbB[:], op=ALU.add),
             reads=[bank_res[lb], cres], writes=[r_res])
        P.op("dve", lambda e: e.max(out=top8[:], in_=lg[:]), reads=[r_res], writes=[r_res])
        P.op("dve", lambda e, t=t: e.tensor_scalar(out=M[:, t, :], in0=lg[:], scalar1=top8[:, 3:4], scalar2=None,
                                                   op0=ALU.is_ge), reads=[r_res], writes=[r_res, G_res[t]])
        P.op("dve", lambda e: e.tensor_scalar(out=rt[:, 0:1], in0=top8[:, 0:1], scalar1=-1.0, scalar2=None,
                                              op0=ALU.mult), reads=[r_res], writes=[r_res])
        P.op("act", lambda e: e.activation(out=ex[:], in_=lg[:], func=AF.Exp, bias=rt[:, 0:1], scale=1.0),
             reads=[r_res], writes=[r_res])
        P.op("dve", lambda e, t=t: e.tensor_tensor(out=ex[:], in0=ex[:], in1=M[:, t, :], op=ALU.mult),
             reads=[r_res, G_res[t]], writes=[r_res])
        P.op("dve", lambda e: e.reduce_sum(out=rt[:, 1:2], in_=ex[:], axis=AX.X), reads=[r_res], writes=[r_res])
        P.op("dve", lambda e: e.reciprocal(out=rt[:, 2:3], in_=rt[:, 1:2]), reads=[r_res], writes=[r_res])
        P.op("dve", lambda e, t=t: e.tensor_scalar(out=G[:, t, :], in0=ex[:], scalar1=rt[:, 2:3], scalar2=None,
                                                   op0=ALU.mult), reads=[r_res, G_res[t]], writes=[G_res[t]])
        jg = t % 4
        if jg > 0:
            P.op("pe", lambda e: e.matmul(bank[:, 4, 0:NE], onesf[:], cm[:], start=True, stop=False),
                 reads=[cm_res, cres], writes=[bank_res[4]])
        P.op("pe", lambda e, t=t, jg=jg: e.matmul(bank[:, 4, 0:NE], Um[:], M[:, t, :], start=(jg == 0), stop=True),
             reads=[G_res[t], cres], writes=[bank_res[4]])
        P.op("act", lambda e, t=t: e.activation(out=R[:, t, :], in_=bank[:, 4, 0:NE], func=AF.Copy),
             reads=[bank_res[4]], writes=[R_res[t]])
        if jg == 0:
            P.op("dve", lambda e, t=t: e.tensor_copy(out=cm[:], in_=M[:, t, :]), reads=[G_res[t]], writes=[cm_res])
        elif jg < 3:
            P.op("dve", lambda e, t=t: e.tensor_tensor(out=cm[:], in0=cm[:], in1=M[:, t, :], op=ALU.add),
                 reads=[G_res[t], cm_res], writes=[cm_res])
        P.op("pe", lambda e, t=t: e.transpose(out=bank[0:NE, 3, 0:128], in_=G[:, t, :], identity=ident[:]),
             reads=[G_res[t], cres], writes=[bank_res[3]])
        P.op("act", lambda e: e.activation(out=GTt[:], in_=bank[0:NE, 3, 0:128], func=AF.Copy),
             reads=[bank_res[3]], writes=[GTt_res])
        for p in range(2):
            bk = 5 + p
            P.op("pe", lambda e, bk=bk, p=p: e.matmul(bank[:, bk, :], GTt[:], b2s[:, p * 512:(p + 1) * 512], start=True, stop=True),
                 reads=[GTt_res, cres], writes=[bank_res[bk]])
            P.op("dve", lambda e, bk=bk, t=t, p=p: e.scalar_tensor_tensor(
                out=xm[:, t, p * 512:(p + 1) * 512], in0=xm[:, t, p * 512:(p + 1) * 512], scalar=ALPHA, in1=bank[:, bk, :],
                op0=ALU.mult, op1=ALU.add),
                reads=[bank_res[bk], xm_res[t], xb_res[t]], writes=[xm_res[t]])

    route_a(0)
    for t in range(NT):
        if t + 1 < NT:
            route_a(t + 1)
        route_b(t)

    NRING = 4
    ring = [P.sb(f"ring{i}", [128, 8, 512], BF16) for i in range(NRING)]
    ring_res = [Res() for _ in range(NRING)]
    xgT = [P.sb(f"xgT{i}", [128, 8, 512], BF16) for i in range(2)]
    xgT_res = [[Res() for _ in range(4)] for _ in range(2)]
    Sel = P.sb("Sel", [128, NT, 128], BF16)
    Sel_res = [Res() for _ in range(4)]
    SelGT = [P.sb(f"SelGT{i}", [128, 4, 512], BF16) for i in range(2)]
    SelGT_res = [[Res() for _ in range(4)] for _ in range(2)]
    tkg = P.sb("tkg", [128, 2], F32)
    tkg_res = Res()
    aT = P.sb("aT", [128, 8, 512], BF16)
    aT_res = [Res() for _ in range(8)]
    ysb = P.sb("ysb", [128, 4, D], BF16)
    ysb_res = [Res() for _ in range(4)]
    tb = [P.sb(f"tb{i}", [128, 512], F32) for i in range(2)]
    tb_res = [Res() for _ in range(2)]
    sg = [P.sb(f"sg{i}", [128, 512], BF16) for i in range(2)]
    sg_res = [Res() for _ in range(2)]
    w1, w2 = dr["w1"], dr["w2"]
    if stage < 4:
        ne_run = 0
    pieces = []
    for ex_i in range(ne_run):
        for p in range(4):
            pieces.append((ex_i, "w1", p))
        for p in range(2):
            pieces.append((ex_i, "w2", p))
    st = {"next_load": 0, "sel": 0, "hb": 0, "tb": 0, "sg": 0, "yb": 0, "cb": 0}

    def load_piece():
        idx = st["next_load"]
        if idx >= len(pieces):
            return
        st["next_load"] += 1
        ex_i, kind, p = pieces[idx]
        slot = idx % NRING
        src = (w1 if kind == "w1" else w2)[ex_i, :, p * 512:(p + 1) * 512].rearrange("(kc q) c -> q kc c", q=128)
        P.dma("pool", lambda e, slot=slot, src=src: e.dma_start(out=ring[slot][:], in_=src), writes=[ring_res[slot]])

    def sel_prep(ex_i):
        P.op("dve", lambda e: e.tensor_copy(out=TB[:, :, 2], in_=G[:, :, ex_i]), reads=G_res, writes=[TB_res])
        P.op("dve", lambda e: e.tensor_tensor(out=TBf[:], in0=G[:, :, ex_i], in1=TB[:, :, 2], op=ALU.subtract),
             reads=G_res + [TB_res], writes=[TB_res])
        P.op("dve", lambda e: e.tensor_copy(out=TB[:, :, 3], in_=TBf[:]), reads=[TB_res], writes=[TB_res])

    def sel_build(ex_i, t):
        P.op("dve", lambda e: e.tensor_scalar(
            out=Sel[:, t, :], in0=iota[:, 0:128], scalar1=R[:, t, ex_i:ex_i + 1], scalar2=M[:, t, ex_i:ex_i + 1],
            op0=ALU.is_equal, op1=ALU.mult), reads=[R_res[t], G_res[t], cres], writes=[Sel_res[t // 4]])

    def gather(ex_i):
        xg, xg_res = xgT[ex_i % 2], xgT_res[ex_i % 2]
        sgt, sgt_res = SelGT[ex_i % 2], SelGT_res[ex_i % 2]
        for g in range(4):
            for hb in range(2):
                bk = hb
                for jj in range(4):
                    dc = hb * 4 + jj
                    for j in range(4):
                        t = g * 4 + j
                        P.op("pe", lambda e, bk=bk, jj=jj, dc=dc, j=j, t=t: e.matmul(
                            bank[:, bk, jj * 128:(jj + 1) * 128], xb[:, t, dc * 128:(dc + 1) * 128], Sel[:, t, :],
                            start=(j == 0), stop=(j == 3)),
                            reads=[xb_res[t], Sel_res[g]], writes=[bank_res[bk]])
                P.op("act", lambda e, hb=hb, bk=bk, g=g, xg=xg: e.activation(
                    out=xg[:, hb * 4:(hb + 1) * 4, g * 128:(g + 1) * 128], in_=bank[:, bk, :].rearrange("p (j c) -> p j c", j=4),
                    func=AF.Copy), reads=[bank_res[bk]], writes=[xg_res[g]])
            for j in range(4):
                t = g * 4 + j
                P.op("pe", lambda e, j=j, t=t: e.matmul(bank[:, 7, 0:4], Sel[:, t, :], TB[:, t, :],
                                                        start=(j == 0), stop=(j == 3)),
                     reads=[Sel_res[g], TB_res], writes=[bank_res[7]])
            P.op("dve", lambda e: e.tensor_scalar(out=tkg[:, 0:1], in0=bank[:, 7, 0:1], scalar1=128.0, scalar2=bank[:, 7, 1:2],
                                                  op0=ALU.mult, op1=ALU.add), reads=[bank_res[7]], writes=[tkg_res])
            P.op("dve", lambda e: e.tensor_scalar(out=tkg[:, 1:2], in0=bank[:, 7, 2:3], scalar1=bank[:, 7, 3:4], scalar2=None,
                                                  op0=ALU.add), reads=[bank_res[7]], writes=[tkg_res])
            P.op("dve", lambda e, g=g, sgt=sgt: e.tensor_scalar(out=sgt[:, g, :], in0=iota[:], scalar1=tkg[:, 0:1], scalar2=tkg[:, 1:2],
                                                              op0=ALU.is_equal, op1=ALU.mult),
                 reads=[tkg_res, cres], writes=[sgt_res[g]])

    def ffn1(ex_i, base):
        xg, xg_res = xgT[ex_i % 2], xgT_res[ex_i % 2]
        nxt = ex_i + 1 if ex_i + 1 < ne_run else None
        if nxt is not None:
            sel_prep(nxt)
        for p in range(4):
            slot = (base + p) % NRING
            for j in range(4):
                fc = p * 4 + j
                bk = 4 + (st["hb"] % 3)
                st["hb"] += 1
                for dc in range(8):
                    P.op("pe", lambda e, bk=bk, slot=slot, j=j, dc=dc, xg=xg: e.matmul(
                        bank[:, bk, :], ring[slot][:, dc, j * 128:(j + 1) * 128], xg[:, dc, :], start=(dc == 0), stop=(dc == 7)),
                        reads=[ring_res[slot]] + xg_res, writes=[bank_res[bk]])
                ti = st["tb"] % 2
                st["tb"] += 1
                b1col = b1T[:, ex_i, fc:fc + 1]
                P.op("dve", lambda e, ti=ti, bk=bk, b1col=b1col: e.tensor_scalar(
                    out=tb[ti][:], in0=bank[:, bk, :], scalar1=b1col, scalar2=7.0, op0=ALU.add, op1=ALU.min),
                    reads=[bank_res[bk], cres], writes=[tb_res[ti]])
                if fc < 8:
                    si = st["sg"] % 2
                    st["sg"] += 1
                    P.op("act", lambda e, ti=ti, si=si: e.activation(out=sg[si][:], in_=tb[ti][:], func=AF.Sigmoid, scale=1.702),
                         reads=[tb_res[ti]], writes=[sg_res[si]])
                    P.op("pool", lambda e, ti=ti, si=si, fc=fc: e.tensor_tensor(out=aT[:, fc, :], in0=tb[ti][:], in1=sg[si][:], op=ALU.mult),
                         reads=[tb_res[ti], sg_res[si]], writes=[aT_res[fc]])
                else:
                    fg = fc - 8
                    P.op("dve", lambda e, ti=ti: e.tensor_scalar(out=tb[ti][:], in0=tb[ti][:], scalar1=-7.0, scalar2=1.0,
                                                                 op0=ALU.max, op1=ALU.add), reads=[tb_res[ti]], writes=[tb_res[ti]])
                    P.op("pool", lambda e, ti=ti, fg=fg: e.tensor_tensor(out=aT[:, fg, :], in0=aT[:, fg, :], in1=tb[ti][:], op=ALU.mult),
                         reads=[tb_res[ti], aT_res[fg]], writes=[aT_res[fg]])
                if nxt is not None:
                    sel_build(nxt, fc)
            load_piece()

    def ffn2(ex_i, base):
        for p in range(2):
            slot = (base + 4 + p) % NRING
            for g in range(4):
                bk = 2 + (st["yb"] % 2)
                st["yb"] += 1
                for fc in range(8):
                    P.op("pe", lambda e, bk=bk, fc=fc, g=g, slot=slot: e.matmul(
                        bank[:, bk, :], aT[:, fc, g * 128:(g + 1) * 128], ring[slot][:, fc, :], start=(fc == 0), stop=(fc == 7)),
                        reads=[ring_res[slot], aT_res[fc]], writes=[bank_res[bk]])
                P.op("act", lambda e, bk=bk, g=g, p=p: e.activation(out=ysb[:, g, p * 512:(p + 1) * 512], in_=bank[:, bk, :], func=AF.Copy),
                     reads=[bank_res[bk]], writes=[ysb_res[g]])
            load_piece()

    def combine(ex_i):
        sgt, sgt_res = SelGT[ex_i % 2], SelGT_res[ex_i % 2]
        for g in range(4):
            for j in range(4):
                t = g * 4 + j
                for hf in range(2):
                    bk = st["cb"] % 4
                    st["cb"] += 1
                    P.op("pe", lambda e, bk=bk, g=g, j=j, hf=hf, sgt=sgt: e.matmul(
                        bank[:, bk, :], sgt[:, g, j * 128:(j + 1) * 128], ysb[:, g, hf * 512:(hf + 1) * 512], start=True, stop=True),
                        reads=[sgt_res[g], ysb_res[g]], writes=[bank_res[bk]])
                    P.op("dve", lambda e, bk=bk, t=t, hf=hf: e.tensor_tensor(
                        out=xm[:, t, hf * 512:(hf + 1) * 512], in0=bank[:, bk, :], in1=xm[:, t, hf * 512:(hf + 1) * 512], op=ALU.add),
                        reads=[bank_res[bk], xm_res[t]], writes=[xm_res[t]])

    for _ in range(3):
        load_piece()
    if ne_run > 0:
        sel_prep(0)
        for t in range(NT):
            sel_build(0, t)
        gather(0)
    for ex_i in range(ne_run):
        ffn1(ex_i, ex_i * 6)
        if ex_i + 1 < ne_run:
            gather(ex_i + 1)
        ffn2(ex_i, ex_i * 6)
        combine(ex_i)
    P.pop_scope()

    P.push_scope()
    lnp = load_const(P, C, "lnp", dr["lnp"][:, 2:4, :], [128, 2, D])
    xTst = [P.sb(f"xTst{i}", [128, 8, 128], BF16) for i in range(2)]
    xTst_res = [Res(), Res()]
    for tt in range(NT):
        if stage >= 6:
            emit_layernorm(P, C, xm[:, tt, :], xm_res[tt], xm[:, tt, :], xm_res[tt], lnp[:, 0, :], lnp[:, 1, :])
        P.dma("sp", lambda e, tt=tt: e.dma_start(out=dr["xout"][tt * 128:(tt + 1) * 128, :], in_=xm[:, tt, :]),
              reads=[xm_res[tt]], writes=_r(dr, "xout_res"))
        if dr.get("xTnext") is not None:
            k = tt % 2
            for hb in range(2):
                bk = 2 + hb
                for j in range(4):
                    dc = hb * 4 + j
                    P.op("pe", lambda e, tt=tt, dc=dc, j=j, bk=bk: e.transpose(
                        out=bank[:, bk, j * 128:(j + 1) * 128], in_=xm[:, tt, dc * 128:(dc + 1) * 128], identity=ident[:]),
                        reads=[xm_res[tt], cres], writes=[bank_res[bk]])
                P.op("act", lambda e, hb=hb, bk=bk, k=k: e.activation(
                    out=xTst[k][:, hb * 4:(hb + 1) * 4, :], in_=bank[:, bk, :].rearrange("p (j c) -> p j c", j=4),
                    func=AF.Copy), reads=[bank_res[bk]], writes=[xTst_res[k]])
            c0 = dr["xTnext_col0"] + tt * 128
            dst = dr["xTnext"][:, c0:c0 + 128].rearrange("(dc p) t -> p dc t", p=128)
            P.dma("sp", lambda e, k=k, dst=dst: e.dma_start(out=dst, in_=xTst[k][:]),
                  reads=[xTst_res[k]], writes=_r(dr, "xTnext_res"))
    P.pop_scope()


NEGBIG = -30000.0


def _r(dr, key):
    r = dr.get(key)
    return [] if r is None else (list(r) if isinstance(r, (list, tuple)) else [r])
HD = 128


def key_chunks(ci):
    out = []
    for c in range(ci + 1):
        for m in range(4):
            out.append((4 * c + m, c == ci))
    for c in range(ci + 1):
        for m in range(4):
            out.append((16 + 4 * c + m, c == ci))
    return out


def mask_index(ci, jl):
    return ci * 8 + (jl % 4 if jl < 16 else 4 + jl % 4)


def emit_mixer_common(P, C, dr):
    cres = C["const_res"]
    qposB = P.sb("qposB", [128, NOWN], F32)
    kposc = P.sb("kposc", [128, 32], F32)
    P.dma("sp", lambda e: e.dma_start(out=qposB[:], in_=dr["qposB"]), writes=[cres])
    P.dma("sp", lambda e: e.dma_start(out=kposc[:], in_=dr["kposc"]), writes=[cres])
    identb = P.sb("identb", [128, 128], BF16)
    onesb = P.sb("onesb", [128, 128], BF16)
    P.op("dve", lambda e: e.tensor_copy(out=identb[:], in_=C["ident"][:]), reads=[cres], writes=[cres])
    P.op("dve", lambda e: e.memset(onesb[:], 1.0), writes=[cres])
    C.update(qposB=qposB, kposc=kposc, identb=identb, onesb=onesb)
    xm, xm_res = C["xm"], C["xm_res"]
    for t in range(NT):
        P.dma("sp", lambda e, t=t: e.dma_start(out=xm[:, t, :], in_=dr["xown"][t * 128:(t + 1) * 128, :]),
              reads=_r(dr, "xown_res"), writes=[xm_res[t]])
        P.op("pool", lambda e, t=t: e.tensor_scalar(out=xm[:, t, :], in0=xm[:, t, :], scalar1=ALPHA, scalar2=1.0,
                                                    op0=ALU.mult, op1=ALU.mult),
             reads=[xm_res[t]], writes=[xm_res[t]])


def emit_masks(P, C):
    C["maskT"] = [P.sb(f"maskT{i}", [128, 8, 512], BF16) for i in range(2)]
    C["mask_res"] = [Res("mask0"), Res("mask1")]
    C["mask_k"] = 0


def gen_masks(P, C, ci):
    i = C["mask_k"] % 2
    C["mask_k"] += 1
    qposB, kposc = C["qposB"], C["kposc"]
    for (jl, masked) in key_chunks(ci):
        if not masked:
            continue
        mi = mask_index(ci, jl) % 8
        P.op("dve", lambda e, ci=ci, jl=jl, mi=mi, i=i: e.tensor_scalar(
            out=C["maskT"][i][:, mi, :], in0=qposB[:, ci * 512:(ci + 1) * 512], scalar1=kposc[:, jl:jl + 1],
            scalar2=NEGBIG, op0=ALU.is_lt, op1=ALU.mult),
            reads=[C["const_res"]], writes=[C["mask_res"][i]])
    return C["maskT"][i], C["mask_res"][i]


class SlabLoader:
    def __init__(self, P, dr, n=2):
        self.P, self.dr, self.n = P, dr, n
        self.buf = [P.sb(f"slab{i}", [128, 8, 512], BF16) for i in range(n)]
        self.res = [Res(f"slab{i}") for i in range(n)]
        self.k = 0

    def load(self, s):
        i = self.k % self.n
        self.k += 1
        pb = self.dr.get("slab_map", list(range(8)))[s]
        src = self.dr["xT"][:, pb * 512:(pb + 1) * 512].rearrange("(dc p) t -> p dc t", p=128)
        self.P.dma("pool", lambda e, i=i, src=src: e.dma_start(out=self.buf[i][:], in_=src),
                   reads=_r(self.dr, "xT_res"), writes=[self.res[i]])
        return self.buf[i], self.res[i]


def emit_attention_mixer(P, C, dr, kind):
    bank, bank_res = C["bank"], C["bank_res"]
    cres = C["const_res"]
    xm, xm_res = C["xm"], C["xm_res"]
    identb, onesb = C["identb"], C["onesb"]
    ident = C["ident"]
    w_in, w_out = dr["w_in"], dr["w_out"]
    NH = 8
    scale = HD ** -0.5
    slabs = SlabLoader(P, dr)
    wh = [P.sb(f"wh{i}", [128, 8, 3, 128], BF16) for i in range(2)]
    wh_res = [Res() for _ in range(2)]
    wo = [P.sb(f"wo{i}", [128, D], BF16) for i in range(2)]
    wo_res = [Res() for _ in range(2)]
    kT = P.sb("kT", [128, S], BF16)
    kT_res = [Res() for _ in range(8)]
    vv = P.sb("vv", [128, 32, 128], BF16)
    vv_res = [Res() for _ in range(8)]
    qT = P.sb("qT", [128, NOWN], BF16)
    qT_res = [Res() for _ in range(4)]
    oT = P.sb("oT", [128, NOWN], BF16)
    oT_res = [Res() for _ in range(4)]
    pT = [P.sb(f"pT{i}", [128, 512], BF16) for i in range(3)]
    pT_res = [Res() for _ in range(3)]
    rs = P.sb("rs", [128, 512], F32)
    rs_res = Res()
    pacc = [P.sb(f"pacc{i}", [128, 512], F32) for i in range(2)]
    pacc_res = [Res(), Res()]
    onesf = P.sb("onesf", [128, 128], F32)
    P.op("dve", lambda e: e.memset(onesf[:], 1.0), writes=[cres])

    def load_head_weights(h):
        i = h % 2
        for j in range(3):
            src = w_in[:, j * D + h * HD: j * D + (h + 1) * HD].rearrange("(dc p) c -> p dc c", p=128)
            P.dma("pool", lambda e, i=i, j=j, src=src: e.dma_start(out=wh[i][:, :, j, :], in_=src), writes=[wh_res[i]])
        src = w_out[h * HD:(h + 1) * HD, :]
        P.dma("pool", lambda e, i=i, src=src: e.dma_start(out=wo[i][:], in_=src), writes=[wo_res[i]])

    if kind == "fox":
        wf = P.sb("wf", [128, 8, 8], BF16)
        P.dma("pool", lambda e: e.dma_start(out=wf[:], in_=w_in[:, 3 * D:3 * D + 8].rearrange("(dc p) c -> p dc c", p=128)),
              writes=[cres])
        bfn = P.sb("bfn", [8, 1], F32)
        P.dma("sp", lambda e: e.dma_start(out=bfn[:], in_=dr["b_f"]), writes=[cres])
        P.op("dve", lambda e: e.tensor_scalar(out=bfn[:], in0=bfn[:], scalar1=-1.0, scalar2=None, op0=ALU.mult),
             reads=[cres], writes=[cres])
        LcT = P.sb("LcT", [8, 64], F32)
        P.dma("sp", lambda e: e.dma_start(out=LcT[:], in_=dr["Lc"]), writes=[cres])
        selh = P.sb("selh", [24, 8, 128], BF16)
        P.dma("pool", lambda e: e.dma_start(out=selh[:], in_=dr["selh"]), writes=[cres])
        ones8 = P.sb("ones8", [8, 512], F32)
        P.op("dve", lambda e: e.memset(ones8[:], 1.0), writes=[cres])
        csp = P.sb("csp", [8, S], F32)
        csp_res = Res()
        e1 = P.sb("e1", [8, 512], F32)
        e1_res = Res()
        tot = P.sb("tot", [8, 8], F32)
        off = P.sb("off", [8, 8], F32)
        tmp8 = P.sb("tmp8", [8, 8], F32)
        for s in range(8):
            sl, sl_res = slabs.load(s)
            for dc in range(8):
                P.op("pe", lambda e, dc=dc, sl=sl: e.matmul(bank[0:8, 0, :], wf[:, dc, :], sl[:, dc, :],
                                                            start=(dc == 0), stop=(dc == 7)),
                     reads=[sl_res, cres], writes=[bank_res[0]])
            P.op("act", lambda e: e.activation(out=e1[:], in_=bank[0:8, 0, :], func=AF.Exp, bias=bfn[:, 0:1], scale=-1.0),
                 reads=[bank_res[0], cres], writes=[e1_res])
            P.op("act", lambda e: e.activation(out=e1[:], in_=e1[:], func=AF.Ln, bias=1.0, scale=1.0),
                 reads=[e1_res], writes=[e1_res])
            P.op("dve", lambda e, s=s: e.tensor_tensor_scan(out=csp[:, s * 512:(s + 1) * 512], data0=ones8[:], data1=e1[:],
                                                           initial=0.0, op0=ALU.mult, op1=ALU.add),
                 reads=[e1_res, cres], writes=[csp_res])
            P.op("dve", lambda e, s=s: e.tensor_copy(out=tot[:, s:s + 1], in_=csp[:, (s + 1) * 512 - 1:(s + 1) * 512]),
                 reads=[csp_res], writes=[csp_res])
        for ci in range(8):
            P.op("dve", lambda e, ci=ci: e.tensor_tensor(out=tmp8[:], in0=tot[:], in1=LcT[:, ci * 8:(ci + 1) * 8], op=ALU.mult),
                 reads=[csp_res, cres], writes=[csp_res])
            P.op("dve", lambda e, ci=ci: e.reduce_sum(out=off[:, ci:ci + 1], in_=tmp8[:], axis=AX.X),
                 reads=[csp_res], writes=[csp_res])
        for ci in range(8):
            P.op("dve", lambda e, ci=ci: e.tensor_scalar(out=csp[:, ci * 512:(ci + 1) * 512], in0=csp[:, ci * 512:(ci + 1) * 512],
                                                         scalar1=off[:, ci:ci + 1], scalar2=None, op0=ALU.add),
                 reads=[csp_res], writes=[csp_res])
        Ck = P.sb("Ck", [128, 32, 8], F32)
        for g in range(2):
            for j in range(16):
                jl = g * 16 + j
                P.op("pe", lambda e, jl=jl, j=j: e.transpose(out=bank[:, 1, j * 8:(j + 1) * 8], in_=csp[:, jl * 128:(jl + 1) * 128],
                                                             identity=ident[0:8, 0:8]),
                     reads=[csp_res, cres], writes=[bank_res[1]])
            P.op("act", lambda e, g=g: e.activation(out=Ck[:, g * 16:(g + 1) * 16, :],
                                                    in_=bank[:, 1, 0:128].rearrange("p (j h) -> p j h", h=8), func=AF.Copy),
                 reads=[bank_res[1]], writes=[csp_res])
        c3 = P.sb("c3", [24, NOWN], BF16)
        c3p = [P.sb(f"c3p{i}", [8, 512], BF16) for i in range(3)]
        r1 = P.sb("r1", [8, 512], F32)
        r2 = P.sb("r2", [8, 512], F32)
        for qc in range(4):
            cs = csp[:, qc * 512:(qc + 1) * 512]
            P.op("dve", lambda e, cs=cs: e.tensor_scalar(out=c3p[0][:], in0=cs, scalar1=-1.0, scalar2=None, op0=ALU.mult),
                 reads=[csp_res], writes=[csp_res])
            P.op("dve", lambda e, cs=cs: e.tensor_tensor(out=r1[:], in0=cs, in1=c3p[0][:], op=ALU.add),
                 reads=[csp_res], writes=[csp_res])
            P.op("dve", lambda e: e.tensor_scalar(out=c3p[1][:], in0=r1[:], scalar1=-1.0, scalar2=None, op0=ALU.mult),
                 reads=[csp_res], writes=[csp_res])
            P.op("dve", lambda e: e.tensor_tensor(out=r2[:], in0=r1[:], in1=c3p[1][:], op=ALU.add),
                 reads=[csp_res], writes=[csp_res])
            P.op("dve", lambda e: e.tensor_scalar(out=c3p[2][:], in0=r2[:], scalar1=-1.0, scalar2=None, op0=ALU.mult),
                 reads=[csp_res], writes=[csp_res])
            for i in range(3):
                P.dma("sp", lambda e, i=i, qc=qc: e.dma_start(out=c3[i * 8:(i + 1) * 8, qc * 512:(qc + 1) * 512], in_=c3p[i][:]),
                      reads=[csp_res], writes=[csp_res])
        fox_res = csp_res
    else:
        esel = P.sb("esel", [16, 16, 128], BF16)
        P.dma("pool", lambda e: e.dma_start(out=esel[:], in_=dr["esel"]), writes=[cres])
        bposB = P.sb("bposB", [128, 16], F32)
        ownb = P.sb("ownb", [128, 16], F32)
        P.dma("sp", lambda e: e.dma_start(out=bposB[:], in_=dr["bposB"]), writes=[cres])
        P.dma("sp", lambda e: e.dma_start(out=ownb[:], in_=dr["ownb"]), writes=[cres])
        kms = P.sb("kms", [128, 16], F32)
        kmT = P.sb("kmT", [128, 16], BF16)
        km_res = Res()
        selbT = P.sb("selbT", [16, NOWN], BF16)
        selb_res = Res()
        gsc = {n: P.sb("g_" + n, [128, 16], F32) for n in ["gm", "sel", "sb"]}
        g8 = P.sb("g_top8", [128, 8], F32)
        g_res = Res()
        pastA = P.sb("g_pastA", [128, NT, 16], F32)
        nbA = P.sb("g_nbA", [128, NT, 16], F32)
        ownA = P.sb("g_ownA", [128, NT, 16], F32)
        for t in range(NT):
            P.op("dve", lambda e, t=t: e.tensor_scalar(out=pastA[:, t, :], in0=bposB[:], scalar1=ownb[:, t:t + 1], scalar2=None,
                                                       op0=ALU.is_lt), reads=[cres], writes=[cres])
            P.op("dve", lambda e, t=t: e.tensor_scalar(out=nbA[:, t, :], in0=pastA[:, t, :], scalar1=-1.0, scalar2=1e30,
                                                       op0=ALU.add, op1=ALU.mult), reads=[cres], writes=[cres])
            P.op("dve", lambda e, t=t: e.tensor_scalar(out=ownA[:, t, :], in0=bposB[:], scalar1=ownb[:, t:t + 1], scalar2=None,
                                                       op0=ALU.is_equal), reads=[cres], writes=[cres])

    load_head_weights(0)
    sT_i = 0
    pT_i = 0
    for h in range(NH):
        wi = h % 2
        if h + 1 < NH:
            load_head_weights(h + 1)
        for s in range(8):
            sl, sl_res = slabs.load(s)
            for dc in range(8):
                P.op("pe", lambda e, dc=dc, sl=sl, wi=wi: e.matmul(bank[:, 0, :], wh[wi][:, dc, 1, :], sl[:, dc, :],
                                                                  start=(dc == 0), stop=(dc == 7)),
                     reads=[sl_res, wh_res[wi]], writes=[bank_res[0]])
            P.op("act", lambda e, s=s: e.activation(out=kT[:, s * 512:(s + 1) * 512], in_=bank[:, 0, :], func=AF.Copy),
                 reads=[bank_res[0]], writes=[kT_res[s]])
            for tt in range(4):
                for dc in range(8):
                    P.op("pe", lambda e, dc=dc, sl=sl, wi=wi, tt=tt: e.matmul(
                        bank[:, 1, tt * 128:(tt + 1) * 128], sl[:, dc, tt * 128:(tt + 1) * 128], wh[wi][:, dc, 2, :],
                        start=(dc == 0), stop=(dc == 7)),
                        reads=[sl_res, wh_res[wi]], writes=[bank_res[1]])
            P.op("act", lambda e, s=s: e.activation(out=vv[:, s * 4:(s + 1) * 4, :],
                                                    in_=bank[:, 1, :].rearrange("p (t c) -> p t c", t=4), func=AF.Copy),
                 reads=[bank_res[1]], writes=[vv_res[s]])
            if s < 4:
                for dc in range(8):
                    P.op("pe", lambda e, dc=dc, sl=sl, wi=wi: e.matmul(bank[:, 0, :], wh[wi][:, dc, 0, :], sl[:, dc, :],
                                                                      start=(dc == 0), stop=(dc == 7)),
                         reads=[sl_res, wh_res[wi]], writes=[bank_res[0]])
                P.op("act", lambda e, s=s: e.activation(out=qT[:, s * 512:(s + 1) * 512], in_=bank[:, 0, :], func=AF.Copy,
                                                        scale=scale),
                     reads=[bank_res[0]], writes=[qT_res[s]])
        if kind == "moba":
            P.op("dve", lambda e: e.tensor_reduce(out=kms[:], in_=kT[:].rearrange("p (n k) -> p n k", k=256), axis=AX.X,
                                                  op=ALU.add),
                 reads=kT_res, writes=[km_res])
            P.op("dve", lambda e: e.tensor_scalar(out=kmT[:], in0=kms[:], scalar1=1.0 / 256.0, scalar2=None, op0=ALU.mult),
                 reads=[km_res], writes=[km_res])
            for t in range(NT):
                P.op("pe", lambda e, t=t: e.matmul(bank[:, 0, 0:16], qT[:, t * 128:(t + 1) * 128], kmT[:], start=True, stop=True),
                     reads=[qT_res[t // 4], km_res], writes=[bank_res[0]])
                G_ = gsc
                P.op("dve", lambda e, t=t: e.tensor_tensor(out=G_["gm"][:], in0=bank[:, 0, 0:16], in1=pastA[:, t, :], op=ALU.mult),
                     reads=[bank_res[0], cres], writes=[g_res])
                P.op("dve", lambda e, t=t: e.tensor_tensor(out=G_["gm"][:], in0=G_["gm"][:], in1=nbA[:, t, :], op=ALU.add),
                     reads=[g_res, cres], writes=[g_res])
                P.op("dve", lambda e: e.max(out=g8[:], in_=G_["gm"][:]), reads=[g_res], writes=[g_res])
                P.op("dve", lambda e: e.tensor_scalar(out=G_["sel"][:], in0=G_["gm"][:], scalar1=g8[:, 2:3], scalar2=None,
                                                      op0=ALU.is_ge), reads=[g_res], writes=[g_res])
                P.op("dve", lambda e, t=t: e.tensor_tensor(out=G_["sel"][:], in0=G_["sel"][:], in1=pastA[:, t, :], op=ALU.mult),
                     reads=[g_res, cres], writes=[g_res])
                P.op("dve", lambda e, t=t: e.tensor_tensor(out=G_["sel"][:], in0=G_["sel"][:], in1=ownA[:, t, :], op=ALU.max),
                     reads=[g_res, cres], writes=[g_res])
                P.op("dve", lambda e: e.tensor_scalar(out=G_["sb"][:], in0=G_["sel"][:], scalar1=-1.0, scalar2=-NEGBIG,
                                                      op0=ALU.add, op1=ALU.mult), reads=[g_res], writes=[g_res])
                P.op("pe", lambda e, t=t: e.transpose(out=bank[0:16, 1, (t % 4) * 128:(t % 4 + 1) * 128], in_=G_["sb"][:],
                                                      identity=ident[:]),
                     reads=[g_res, cres], writes=[bank_res[1]])
                if t % 4 == 3:
                    P.op("act", lambda e, t=t: e.activation(out=selbT[:, (t - 3) * 128:(t + 1) * 128], in_=bank[0:16, 1, :],
                                                            func=AF.Copy),
                         reads=[bank_res[1]], writes=[selb_res])
        for ci in range(4):
            ob = 4 + (ci % 2)
            sb_ = 6 + (ci % 2)
            kcs = key_chunks(ci)
            maskT, mask_res = gen_masks(P, C, ci)
            ai = ci % 2
            nk = len(kcs)
            tiles = []
            for n, (jl, masked) in enumerate(kcs):
                tiles.append(dict(n=n, jl=jl, masked=masked, sbk=2 + (sT_i % 2), pi=pT_i % 3))
                sT_i += 1
                pT_i += 1

            def stage1(T, ci=ci, h=h, maskT=maskT, mask_res=mask_res):
                jl, masked, sbk, pi = T["jl"], T["masked"], T["sbk"], T["pi"]
                P.op("pe", lambda e: e.matmul(
                    bank[:, sbk, :], kT[:, jl * 128:(jl + 1) * 128], qT[:, ci * 512:(ci + 1) * 512], start=True, stop=False),
                    reads=[kT_res[jl // 4], qT_res[ci]], writes=[bank_res[sbk]])
                if kind == "fox":
                    P.op("pe", lambda e: e.matmul(
                        bank[:, sbk, :], selh[:, h, :], c3[:, ci * 512:(ci + 1) * 512], start=False, stop=(not masked)),
                        reads=[fox_res, cres], writes=[bank_res[sbk]])
                else:
                    P.op("pe", lambda e: e.matmul(
                        bank[:, sbk, :], esel[:, jl // 2, :], selbT[:, ci * 512:(ci + 1) * 512], start=False, stop=(not masked)),
                        reads=[selb_res, cres], writes=[bank_res[sbk]])
                if masked:
                    mi = mask_index(ci, jl) % 8
                    P.op("pe", lambda e: e.matmul(bank[:, sbk, :], identb[:], maskT[:, mi, :], start=False, stop=True),
                         reads=[mask_res, cres], writes=[bank_res[sbk]])
                if kind == "fox":
                    P.op("act", lambda e: e.activation(
                        out=pT[pi][:], in_=bank[:, sbk, :], func=AF.Exp, bias=Ck[:, jl, h:h + 1], scale=1.0),
                        reads=[bank_res[sbk], fox_res], writes=[pT_res[pi]])
                else:
                    P.op("act", lambda e: e.activation(out=pT[pi][:], in_=bank[:, sbk, :], func=AF.Exp),
                         reads=[bank_res[sbk]], writes=[pT_res[pi]])

            def stage2(T, ob=ob, ai=ai, nk=nk):
                n, jl, pi = T["n"], T["jl"], T["pi"]
                P.op("pe", lambda e: e.matmul(
                    bank[:, ob, :], vv[:, jl, :], pT[pi][:], start=(n == 0), stop=(n == nk - 1)),
                    reads=[pT_res[pi], vv_res[jl // 4]], writes=[bank_res[ob]])
                if n == 0:
                    P.op("dve", lambda e: e.tensor_copy(out=pacc[ai][:], in_=pT[pi][:]),
                         reads=[pT_res[pi]], writes=[pacc_res[ai]])
                else:
                    P.op("dve", lambda e: e.tensor_tensor(out=pacc[ai][:], in0=pacc[ai][:], in1=pT[pi][:], op=ALU.add),
                         reads=[pT_res[pi], pacc_res[ai]], writes=[pacc_res[ai]])

            stage1(tiles[0])
            for n in range(nk):
                if n + 1 < nk:
                    stage1(tiles[n + 1])
                stage2(tiles[n])
            P.op("pe", lambda e, sb_=sb_, ai=ai: e.matmul(bank[:, sb_, :], onesf[:], pacc[ai][:], start=True, stop=True),
                 reads=[pacc_res[ai], cres], writes=[bank_res[sb_]])
            P.op("dve", lambda e, sb_=sb_: e.reciprocal(out=rs[:], in_=bank[:, sb_, :]), reads=[bank_res[sb_]], writes=[rs_res])
            P.op("dve", lambda e, ob=ob, ci=ci: e.tensor_tensor(out=oT[:, ci * 512:(ci + 1) * 512], in0=bank[:, ob, :], in1=rs[:],
                                                                op=ALU.mult),
                 reads=[bank_res[ob], rs_res], writes=[oT_res[ci]])
        for t in range(NT):
            for hf in range(2):
                bk = hf
                P.op("pe", lambda e, t=t, hf=hf, bk=bk, wi=wi: e.matmul(
                    bank[:, bk, :], oT[:, t * 128:(t + 1) * 128], wo[wi][:, hf * 512:(hf + 1) * 512], start=True, stop=True),
                    reads=[oT_res[t // 4], wo_res[wi]], writes=[bank_res[bk]])
                P.op("dve", lambda e, t=t, hf=hf, bk=bk: e.tensor_tensor(
                    out=xm[:, t, hf * 512:(hf + 1) * 512], in0=bank[:, bk, :], in1=xm[:, t, hf * 512:(hf + 1) * 512], op=ALU.add),
                    reads=[bank_res[bk], xm_res[t]], writes=[xm_res[t]])


def emit_retention_mixer(P, C, dr):
    bank, bank_res = C["bank"], C["bank_res"]
    cres = C["const_res"]
    xm, xm_res = C["xm"], C["xm_res"]
    qposB, kposc, ident = C["qposB"], C["kposc"], C["ident"]
    w_in, w_out = dr["w_in"], dr["w_out"]
    NH, DK, DV = 4, 256, 512
    slabs = SlabLoader(P, dr)
    wA = P.sb("wA", [128, 8, 512], BF16)
    wV = P.sb("wV", [128, 8, 512], BF16)
    wG = P.sb("wG", [128, 8, 512], BF16)
    wo = P.sb("wo", [128, 4, D], BF16)
    gng = P.sb("gng", [128, 512], F32)
    wA_res, wV_res, wG_res, wo_res, gng_res = Res(), Res(), Res(), Res(), Res()
    kT = P.sb("kT", [128, 2, S], BF16)
    kT_res = [Res() for _ in range(8)]
    vv = P.sb("vv", [128, 32, 512], BF16)
    vv_res = [Res() for _ in range(8)]
    qT = P.sb("qT", [128, 2, 512], BF16)
    qT_res = Res()
    gs = P.sb("gs", [128, 4, 512], BF16)
    gs_res = [Res() for _ in range(4)]
    oT = P.sb("oT", [128, 4, 512], BF16)
    oT_res = [Res() for _ in range(4)]
    ts = [P.sb(f"ts{i}", [128, 512], F32) for i in range(2)]
    ts_res = [Res(), Res()]
    cs = P.sb("cs", [128, 2, 512], F32)
    cs_res = Res()
    ta = P.sb("ta", [128, 512], F32)
    tb2 = P.sb("tb2", [128, 512], F32)
    ta_res, tb_res = Res(), Res()
    dm, dm_res = ts, ts_res
    pen, pen_res = ta, ta_res
    Dt = [tb2, P.sb("Dt1", [128, 512], F32)]
    Dt_res = [tb_res, Res()]
    pT = [P.sb(f"pT{i}", [128, 512], BF16) for i in range(3)]
    pT_res = [Res() for _ in range(3)]
    on = [P.sb(f"on{i}", [128, 512], F32) for i in range(2)]
    on_res = [Res(), Res()]
    gst = P.sb("gst", [128, 6], F32)
    gmv = P.sb("gmv", [128, 2], F32)
    gsd = P.sb("gsd", [128, 4], F32)
    gn_res = Res()

    def load_head_weights(h):
        def ld(dst, dres, c0, n, off=0):
            src = w_in[:, c0:c0 + n].rearrange("(dc p) c -> p dc c", p=128)
            P.dma("pool", lambda e: e.dma_start(out=dst[:, :, off:off + n], in_=src), writes=[dres])
        ld(wA, wA_res, h * DK, DK, 0)
        ld(wA, wA_res, D + h * DK, DK, DK)
        ld(wV, wV_res, 2 * D + h * DV, DV)
        ld(wG, wG_res, 4 * D + h * DV, DV)
        src = w_out[h * DV:(h + 1) * DV, :].rearrange("(c p) n -> p c n", p=128)
        P.dma("pool", lambda e: e.dma_start(out=wo[:], in_=src), writes=[wo_res])
        P.dma("sp", lambda e: e.dma_start(out=gng[:], in_=dr["gngB"][:, h * DV:(h + 1) * DV]), writes=[gng_res])

    def project_rot(sl, sl_res, s, col0, scale, dst, dst_res):
        for c in range(2):
            for dc in range(8):
                P.op("pe", lambda e, c=c, dc=dc: e.matmul(bank[:, c, :], wA[:, dc, col0 + c * 128:col0 + (c + 1) * 128], sl[:, dc, :],
                                                          start=(dc == 0), stop=(dc == 7)),
                     reads=[sl_res, wA_res], writes=[bank_res[c]])
            P.op("act", lambda e, c=c: e.activation(out=ts[c][:], in_=bank[:, c, :], func=AF.Copy, scale=scale),
                 reads=[bank_res[c]], writes=[ts_res[c]])
        P.dma("sp", lambda e: e.dma_start(out=cs[:, 0, :], in_=dr["cosT"][:, s * 512:(s + 1) * 512]), writes=[cs_res])
        P.dma("sp", lambda e: e.dma_start(out=cs[:, 1, :], in_=dr["sinT"][:, s * 512:(s + 1) * 512]), writes=[cs_res])
        P.op("dve", lambda e: e.tensor_tensor(out=ta[:], in0=ts[0][:], in1=cs[:, 0, :], op=ALU.mult),
             reads=[ts_res[0], cs_res], writes=[ta_res])
        P.op("pool", lambda e: e.tensor_tensor(out=tb2[:], in0=ts[1][:], in1=cs[:, 1, :], op=ALU.mult),
             reads=[ts_res[1], cs_res], writes=[tb_res])
        P.op("dve", lambda e: e.tensor_tensor(out=dst[0], in0=ta[:], in1=tb2[:], op=ALU.subtract),
             reads=[ta_res, tb_res], writes=[dst_res])
        P.op("dve", lambda e: e.tensor_tensor(out=ta[:], in0=ts[0][:], in1=cs[:, 1, :], op=ALU.mult),
             reads=[ts_res[0], cs_res], writes=[ta_res])
        P.op("dve", lambda e: e.tensor_tensor(out=tb2[:], in0=ts[1][:], in1=cs[:, 0, :], op=ALU.mult),
             reads=[ts_res[1], cs_res], writes=[tb_res])
        P.op("pool", lambda e: e.tensor_tensor(out=dst[1], in0=ta[:], in1=tb2[:], op=ALU.add),
             reads=[ta_res, tb_res], writes=[dst_res])

    sT_i = pT_i = dm_i = on_i = 0
    for h in range(NH):
        lg = float(np.log(1.0 - 2.0 ** (-5.0 - h)))
        load_head_weights(h)
        for s in range(8):
            sl, sl_res = slabs.load(s)
            project_rot(sl, sl_res, s, DK, DK ** -0.5,
                        (kT[:, 0, s * 512:(s + 1) * 512], kT[:, 1, s * 512:(s + 1) * 512]), kT_res[s])
            for tt in range(4):
                bk = tt % 2
                for dc in range(8):
                    P.op("pe", lambda e, dc=dc, tt=tt, bk=bk, sl=sl: e.matmul(bank[:, bk, :], sl[:, dc, tt * 128:(tt + 1) * 128], wV[:, dc, :],
                                                                            start=(dc == 0), stop=(dc == 7)),
                         reads=[sl_res, wV_res], writes=[bank_res[bk]])
                P.op("act", lambda e, s=s, tt=tt, bk=bk: e.activation(out=vv[:, s * 4 + tt, :], in_=bank[:, bk, :], func=AF.Copy),
                     reads=[bank_res[bk]], writes=[vv_res[s]])
        for ci in range(4):
            sl, sl_res = slabs.load(ci)
            project_rot(sl, sl_res, ci, 0, 1.0, (qT[:, 0, :], qT[:, 1, :]), qT_res)
            kcs = key_chunks(ci)
            nk = len(kcs)
            tiles = []
            for n, (jl, masked) in enumerate(kcs):
                tiles.append(dict(n=n, jl=jl, masked=masked, sbk=2 + (sT_i % 2), di=dm_i % 2, pi=pT_i % 3))
                sT_i += 1
                dm_i += 1
                pT_i += 1

            def stage1(T, ci=ci, lg=lg):
                jl, masked, sbk, di, pi = T["jl"], T["masked"], T["sbk"], T["di"], T["pi"]
                for c in range(2):
                    P.op("pe", lambda e, c=c: e.matmul(bank[:, sbk, :], kT[:, c, jl * 128:(jl + 1) * 128], qT[:, c, :],
                                                       start=(c == 0), stop=(c == 1)),
                         reads=[kT_res[jl // 4], qT_res], writes=[bank_res[sbk]])
                P.op("dve", lambda e: e.tensor_scalar(
                    out=dm[di][:], in0=qposB[:, ci * 512:(ci + 1) * 512], scalar1=kposc[:, jl:jl + 1], scalar2=None,
                    op0=ALU.subtract), reads=[cres], writes=[dm_res[di]])
                if masked:
                    P.op("dve", lambda e: e.tensor_scalar(out=pen[:], in0=dm[di][:], scalar1=0.0, scalar2=1.0e6,
                                                          op0=ALU.is_lt, op1=ALU.mult),
                         reads=[dm_res[di]], writes=[pen_res])
                    P.op("dve", lambda e: e.tensor_tensor(out=dm[di][:], in0=dm[di][:], in1=pen[:], op=ALU.add),
                         reads=[dm_res[di], pen_res], writes=[dm_res[di]])
                P.op("act", lambda e: e.activation(out=Dt[di][:], in_=dm[di][:], func=AF.Exp, scale=lg),
                     reads=[dm_res[di]], writes=[Dt_res[di]])
                P.op("dve", lambda e: e.tensor_tensor(out=pT[pi][:], in0=bank[:, sbk, :], in1=Dt[di][:], op=ALU.mult),
                     reads=[bank_res[sbk], Dt_res[di]], writes=[pT_res[pi]])

            def stage2(T, nk=nk):
                n, jl, pi = T["n"], T["jl"], T["pi"]
                for sub in range(4):
                    P.op("pe", lambda e, sub=sub: e.matmul(
                        bank[:, 4 + sub, :], pT[pi][:, sub * 128:(sub + 1) * 128], vv[:, jl, :], start=(n == 0), stop=(n == nk - 1)),
                        reads=[pT_res[pi], vv_res[jl // 4]], writes=[bank_res[4 + sub]])

            stage1(tiles[0])
            for n in range(nk):
                if n + 1 < nk:
                    stage1(tiles[n + 1])
                stage2(tiles[n])
            for tt in range(4):
                bk = tt % 2
                for dc in range(8):
                    P.op("pe", lambda e, dc=dc, tt=tt, bk=bk, sl=sl: e.matmul(bank[:, bk, :], sl[:, dc, tt * 128:(tt + 1) * 128], wG[:, dc, :],
                                                                            start=(dc == 0), stop=(dc == 7)),
                         reads=[sl_res, wG_res], writes=[bank_res[bk]])
                P.op("act", lambda e, tt=tt, bk=bk: e.activation(out=gs[:, tt, :], in_=bank[:, bk, :], func=AF.Silu),
                     reads=[bank_res[bk]], writes=[gs_res[tt]])
            for sub in range(4):
                ob = 4 + sub
                oi = on_i % 2
                on_i += 1
                P.op("dve", lambda e, ob=ob: e.bn_stats(out=gst[:, 0:6], in_=bank[:, ob, :]), reads=[bank_res[ob]], writes=[gn_res])
                P.op("dve", lambda e: e.bn_aggr(out=gmv[:, 0:2], in_=gst[:, 0:6]), reads=[gn_res], writes=[gn_res])
                P.op("dve", lambda e: e.tensor_scalar(out=gsd[:, 0:1], in0=gmv[:, 1:2], scalar1=1e-6, scalar2=None, op0=ALU.add),
                     reads=[gn_res], writes=[gn_res])
                P.op("act", lambda e: e.activation(out=gsd[:, 1:2], in_=gsd[:, 0:1], func=AF.Sqrt), reads=[gn_res], writes=[gn_res])
                P.op("dve", lambda e: e.reciprocal(out=gsd[:, 2:3], in_=gsd[:, 1:2]), reads=[gn_res], writes=[gn_res])
                P.op("dve", lambda e, ob=ob, oi=oi: e.tensor_scalar(out=on[oi][:], in0=bank[:, ob, :], scalar1=gmv[:, 0:1], scalar2=gsd[:, 2:3],
                                                                   op0=ALU.subtract, op1=ALU.mult),
                     reads=[bank_res[ob], gn_res], writes=[on_res[oi]])
                P.op("pool", lambda e, oi=oi: e.tensor_tensor(out=on[oi][:], in0=on[oi][:], in1=gng[:], op=ALU.mult),
                     reads=[on_res[oi], gng_res], writes=[on_res[oi]])
                P.op("pool", lambda e, oi=oi, sub=sub: e.tensor_tensor(out=on[oi][:], in0=on[oi][:], in1=gs[:, sub, :], op=ALU.mult),
                     reads=[on_res[oi], gs_res[sub]], writes=[on_res[oi]])
                bk = sub % 2
                for c in range(4):
                    P.op("pe", lambda e, c=c, bk=bk, oi=oi: e.transpose(out=bank[:, bk, c * 128:(c + 1) * 128], in_=on[oi][:, c * 128:(c + 1) * 128],
                                                                       identity=ident[:]),
                         reads=[on_res[oi], cres], writes=[bank_res[bk]])
                P.op("act", lambda e, bk=bk, sub=sub: e.activation(out=oT[:, :, sub * 128:(sub + 1) * 128],
                                                                   in_=bank[:, bk, :].rearrange("p (c q) -> p c q", c=4), func=AF.Copy),
                     reads=[bank_res[bk]], writes=[oT_res[sub]])
            for sub in range(4):
                t = ci * 4 + sub
                for hf in range(2):
                    bk = hf
                    for c in range(4):
                        P.op("pe", lambda e, c=c, sub=sub, hf=hf, bk=bk: e.matmul(
                            bank[:, bk, :], oT[:, c, sub * 128:(sub + 1) * 128], wo[:, c, hf * 512:(hf + 1) * 512],
                            start=(c == 0), stop=(c == 3)),
                            reads=[oT_res[sub], wo_res], writes=[bank_res[bk]])
                    P.op("dve", lambda e, t=t, hf=hf, bk=bk: e.tensor_tensor(
                        out=xm[:, t, hf * 512:(hf + 1) * 512], in0=bank[:, bk, :], in1=xm[:, t, hf * 512:(hf + 1) * 512], op=ALU.add),
                        reads=[bank_res[bk], xm_res[t]], writes=[xm_res[t]])


def emit_mixer(P, C, dr, kind):
    P.push_scope()
    emit_mixer_common(P, C, dr)
    if kind in ("fox", "moba"):
        emit_masks(P, C)
        emit_attention_mixer(P, C, dr, kind)
    else:
        emit_retention_mixer(P, C, dr)
    P.pop_scope()
    P.push_scope()
    lnp1 = P.sb("lnp1", [128, 2, D], F32)
    P.dma("sp", lambda e: e.dma_start(out=lnp1[:], in_=dr["lnp"][:, 0:2, :]), writes=[C["const_res"]])
    xm, xm_res = C["xm"], C["xm_res"]
    for t in range(NT):
        emit_layernorm(P, C, xm[:, t, :], xm_res[t], xm[:, t, :], xm_res[t], lnp1[:, 0, :], lnp1[:, 1, :])
    P.pop_scope()


def load_const(P, C, name, dram_ap, shape, dtype=F32, queue="sp"):
    t = P.sb(name, shape, dtype)
    P.dma(queue, lambda e: e.dma_start(out=t[:], in_=dram_ap), reads=[], writes=[C["const_res"]])
    C[name] = t
    return t


def build_program(kind, ne_run=NE, stage=9, mixer_only=False):
    nc = bass.Bass("TRN2", target_bir_lowering=False)
    P = Prog(nc)
    C = {}
    dr = {}

    def din(name, shape):
        dr[name] = nc.dram_tensor(name, list(shape), F32, kind="ExternalInput").ap()
        return dr[name]

    dr["xout"] = nc.dram_tensor("xout", [NOWN, D], F32, kind="ExternalOutput").ap()
    din("ident", [128, 128])
    din("lnp", [128, 4, D])
    din("rw", [D, NE])
    din("rbB", [128, NE])
    din("w1", [ne_run, D, 2 * D])
    din("b1T", [128, NE, 16])
    din("w2", [ne_run, D, D])
    din("b2s", [NE, D])
    din("Umat", [128, 128])
    din("iota", [128, 512])
    din("jp", [128, NT, 2])
    if kind == "moe":
        din("xmid", [NOWN, D])
    else:
        din("xown", [NOWN, D])
        din("xT", [D, S])
        din("qposB", [128, NOWN])
        din("kposc", [128, 32])
    if kind == "fox":
        din("w_in", [D, 3 * D + 8])
        din("w_out", [D, D])
        din("b_f", [8, 1])
        din("Lc", [8, 64])
        din("selh", [24, 8, 128])
    elif kind == "moba":
        din("w_in", [D, 3 * D])
        din("w_out", [D, D])
        din("esel", [16, 16, 128])
        din("bposB", [128, 16])
        din("ownb", [128, 16])
    elif kind == "ret":
        din("w_in", [D, 6 * D])
        din("w_out", [2 * D, D])
        din("gngB", [128, 2 * D])
        din("cosT", [128, S])
        din("sinT", [128, S])

    C["const_res"] = Res("const")
    C["bank"] = P.ps("bank", [128, 8, 512], F32)
    C["bank_res"] = [Res(f"bank{i}") for i in range(8)]
    C["ln_st"] = P.sb("ln_st", [128, 12], F32)
    C["ln_mv"] = P.sb("ln_mv", [128, 2], F32)
    C["ln_sd"] = P.sb("ln_sd", [128, 4], F32)
    C["ln_res"] = Res("ln")
    load_const(P, C, "ident", dr["ident"], [128, 128])
    C["xm"] = P.sb("xm", [128, NT, D], F32)
    C["xm_res"] = [Res(f"xm{t}") for t in range(NT)]

    if kind == "moe":
        for t in range(NT):
            P.dma("sp", lambda e, t=t: e.dma_start(out=C["xm"][:, t, :], in_=dr["xmid"][t * 128:(t + 1) * 128, :]),
                  reads=[], writes=[C["xm_res"][t]])
    else:
        emit_mixer(P, C, dr, kind)
    if mixer_only:
        for tt in range(NT):
            P.dma("sp", lambda e, tt=tt: e.dma_start(out=dr["xout"][tt * 128:(tt + 1) * 128, :], in_=C["xm"][:, tt, :]),
                  reads=[C["xm_res"][tt]], writes=[])
    else:
        emit_moe(P, C, dr, ne_run, stage)
    P.finish()
    return nc


KINDS = ["fox", "moba", "ret", "fox"]
PAIR_ORDER = np.concatenate([np.arange(c * 512, (c + 1) * 512) for c in OWN_CHUNKS[0] + OWN_CHUNKS[1]])


def build_fused(n_layers=4, ne_run=NE):
    nc = bass.Bass("TRN2", target_bir_lowering=False)
    P = Prog(nc)
    C = {}
    E = {}

    def din(name, shape):
        E[name] = nc.dram_tensor(name, list(shape), F32, kind="ExternalInput").ap()
        return E[name]

    for z in range(2):
        din(f"xown{z}", [NOWN, D])
        din(f"qposB{z}", [128, NOWN])
        din(f"kposc{z}", [128, 32])
        din(f"Lc{z}", [8, 64])
        din(f"bposB{z}", [128, 16])
        din(f"ownb{z}", [128, 16])
        din(f"cosT{z}", [128, S])
        din(f"sinT{z}", [128, S])
    din("xT0", [D, S])
    din("ident", [128, 128])
    din("selh", [24, 8, 128])
    din("esel", [16, 16, 128])
    din("fox_w_in", [2, D, 3 * D + 8])
    din("fox_b_f", [2, 8, 1])
    din("fox_w_out", [2, D, D])
    din("moba_w_in", [1, D, 3 * D])
    din("moba_w_out", [1, D, D])
    din("ret_w_in", [1, D, 6 * D])
    din("ret_w_out", [1, 2 * D, D])
    din("gngB", [128, 2 * D])
    din("lnp", [4, 128, 4, D])
    din("rw", [4, D, NE])
    din("rbB", [4, 128, NE])
    din("w1", [4, ne_run, D, 2 * D])
    din("b1T", [4, 128, NE, 16])
    din("w2", [4, ne_run, D, D])
    din("b2s", [4, NE, D])
    din("Umat", [128, 128])
    din("iota", [128, 512])
    din("jp", [128, NT, 2])
    xout = nc.dram_tensor("xout", [S, D], F32, kind="ExternalOutput").ap()
    xs = {}
    xTs = {}
    for L in range(1, n_layers):
        for z in range(2):
            xs[(L, z)] = (nc.dram_tensor(f"xs{L}_{z}", [NOWN, D], F32, kind="Internal").ap(), Res())
        xTs[L] = (nc.dram_tensor(f"xTs{L}", [D, S], BF16, kind="Internal").ap(), [Res(), Res()])

    C["const_res"] = Res("const")
    C["bank"] = P.ps("bank", [128, 8, 512], F32)
    C["bank_res"] = [Res(f"bank{i}") for i in range(8)]
    C["ln_st"] = P.sb("ln_st", [128, 12], F32)
    C["ln_mv"] = P.sb("ln_mv", [128, 2], F32)
    C["ln_sd"] = P.sb("ln_sd", [128, 4], F32)
    C["ln_res"] = Res("ln")
    load_const(P, C, "ident", E["ident"], [128, 128])

    for L in range(n_layers):
        kind = KINDS[L]
        j = L // 3
        for z in range(2):
            dr = {"qposB": E[f"qposB{z}"], "kposc": E[f"kposc{z}"], "Lc": E[f"Lc{z}"], "bposB": E[f"bposB{z}"],
                  "ownb": E[f"ownb{z}"], "cosT": E[f"cosT{z}"], "sinT": E[f"sinT{z}"], "selh": E["selh"], "esel": E["esel"],
                  "gngB": E["gngB"], "lnp": E["lnp"][L], "rw": E["rw"][L], "rbB": E["rbB"][L], "w1": E["w1"][L],
                  "b1T": E["b1T"][L], "w2": E["w2"][L], "b2s": E["b2s"][L],
                  "Umat": E["Umat"], "iota": E["iota"], "jp": E["jp"],
                  "slab_map": [z * 4 + s for s in range(4)] + [(1 - z) * 4 + s for s in range(4)]}
            if kind == "fox":
                dr.update(w_in=E["fox_w_in"][j], w_out=E["fox_w_out"][j], b_f=E["fox_b_f"][j])
            elif kind == "moba":
                dr.update(w_in=E["moba_w_in"][j], w_out=E["moba_w_out"][j])
            else:
                dr.update(w_in=E["ret_w_in"][j], w_out=E["ret_w_out"][j])
            if L == 0:
                dr.update(xown=E[f"xown{z}"], xT=E["xT0"])
            else:
                dr.update(xown=xs[(L, z)][0], xown_res=xs[(L, z)][1], xT=xTs[L][0], xT_res=xTs[L][1])
            if L == n_layers - 1:
                dr.update(xout=xout[z * NOWN:(z + 1) * NOWN, :])
            else:
                dr.update(xout=xs[(L + 1, z)][0], xout_res=xs[(L + 1, z)][1],
                          xTnext=xTs[L + 1][0], xTnext_col0=z * NOWN, xTnext_res=xTs[L + 1][1][z])
            P.push_scope()
            C["xm"] = P.sb("xm", [128, NT, D], F32)
            C["xm_res"] = [Res(f"xm{t}") for t in range(NT)]
            emit_mixer(P, C, dr, kind)
            emit_moe(P, C, dr, ne_run)
            P.pop_scope()
    P.finish()
    return nc


def fused_inputs(inputs, ne_run=NE):
    m = {"ident": np.eye(128, dtype=np.float32)}
    m.update(moe_consts())
    for z in range(2):
        t = mixer_inputs("fox", 0, z, None, inputs, tables_only=True)
        t.update(mixer_inputs("moba", 0, z, None, inputs, tables_only=True))
        t.update(mixer_inputs("ret", 0, z, None, inputs, tables_only=True))
        for k in ["qposB", "kposc", "Lc", "bposB", "ownb", "cosT", "sinT"]:
            m[f"{k}{z}"] = t[k]
        m["selh"], m["esel"] = t["selh"], t["esel"]
    m["fox_w_in"] = np.ascontiguousarray(inputs["fox_w_in"])
    m["fox_b_f"] = np.ascontiguousarray(inputs["fox_b_f"].reshape(2, 8, 1))
    m["fox_w_out"] = np.ascontiguousarray(inputs["fox_w_out"])
    m["moba_w_in"] = np.ascontiguousarray(inputs["moba_w_in"])
    m["moba_w_out"] = np.ascontiguousarray(inputs["moba_w_out"])
    m["ret_w_in"] = np.ascontiguousarray(inputs["ret_w_in"])
    m["ret_w_out"] = np.ascontiguousarray(inputs["ret_w_out"])
    m["gngB"] = np.ascontiguousarray(np.broadcast_to(inputs["ret_gn_g"][0][None], (128, 2 * D)))
    lnp = np.stack([inputs["ln_g"][:, 0], inputs["ln_b"][:, 0], inputs["ln_g"][:, 1], inputs["ln_b"][:, 1]], 1)
    m["lnp"] = np.ascontiguousarray(np.broadcast_to(lnp[:, None], (4, 128, 4, D)))
    m["rw"] = np.ascontiguousarray(inputs["router_w"])
    m["rbB"] = np.ascontiguousarray(np.broadcast_to(inputs["router_b"][:, None, :], (4, 128, NE)))
    m["w1"] = np.ascontiguousarray(inputs["moe_w1"][:, :ne_run])
    m["b1T"] = np.ascontiguousarray(inputs["moe_b1"].reshape(4, NE, 16, 128).transpose(0, 3, 1, 2))
    m["w2"] = np.ascontiguousarray(inputs["moe_w2"][:, :ne_run])
    m["b2s"] = np.ascontiguousarray(inputs["moe_b2"])
    return {k: np.asarray(v, dtype=np.float32) for k, v in m.items()}


_FUSED = {}


def run_fused(inputs, n_layers=4, cores=NB, ne_run=NE):
    key = (n_layers, ne_run)
    if key not in _FUSED:
        _FUSED[key] = build_fused(n_layers, ne_run)
    nc = _FUSED[key]
    shared = fused_inputs(inputs, ne_run)
    x = inputs["x"]
    in_maps = []
    for b in range(cores):
        m = dict(shared)
        for z in range(2):
            m[f"xown{z}"] = np.ascontiguousarray(x[b][own_index(z)])
        m["xT0"] = np.ascontiguousarray(x[b][PAIR_ORDER].T)
        in_maps.append(m)
    res = run_bass_kernel_spmd(nc, in_maps, core_ids=list(range(cores)))
    out = np.zeros((cores, S, D), np.float32)
    for b in range(cores):
        out[b][PAIR_ORDER] = res.results[b]["xout"]
    return out


def kernel(**inputs):
    inputs = {k: np.asarray(v) for k, v in inputs.items()}
    return run_fused(inputs, 4, NB, NE)


def moe_consts():
    k = np.arange(128)
    jp = np.zeros((128, NT, 2), np.float32)
    jp[:, :, 0] = (np.arange(NT) % 4)[None, :]
    jp[:, :, 1] = k[:, None]
    return {"Umat": (k[:, None] < k[None, :]).astype(np.float32),
            "iota": np.ascontiguousarray(np.broadcast_to(np.arange(512, dtype=np.float32)[None], (128, 512))),
            "jp": jp}


def moe_inputs(i, inputs):
    b1 = inputs["moe_b1"][i]
    b1T = np.ascontiguousarray(b1.reshape(NE, 16, 128).transpose(2, 0, 1))
    lnp = np.stack([inputs["ln_g"][i, 0], inputs["ln_b"][i, 0], inputs["ln_g"][i, 1], inputs["ln_b"][i, 1]], 0)
    return {
        "ident": np.eye(128, dtype=np.float32),
        "lnp": np.ascontiguousarray(np.broadcast_to(lnp[None], (128, 4, D))).astype(np.float32),
        "rw": np.ascontiguousarray(inputs["router_w"][i]),
        "rbB": np.ascontiguousarray(np.broadcast_to(inputs["router_b"][i][None], (128, NE))).astype(np.float32),
        "w1": np.ascontiguousarray(inputs["moe_w1"][i]),
        "b1T": b1T.astype(np.float32),
        "w2": np.ascontiguousarray(inputs["moe_w2"][i]),
        "b2s": np.ascontiguousarray(inputs["moe_b2"][i]),
    }


def local_pos(z):
    own = OWN_CHUNKS[z]
    peer = OWN_CHUNKS[1 - z]
    return np.concatenate([np.arange(c * 512, (c + 1) * 512) for c in own + peer])


def mixer_inputs(kind, j, z, xb, inputs, tables_only=False):
    pos = local_pos(z)
    m = {} if tables_only else {"xown": np.ascontiguousarray(xb[pos[:NOWN]]), "xT": np.ascontiguousarray(xb[pos].T)}
    m.update({
        "qposB": np.ascontiguousarray(np.broadcast_to(pos[None, :NOWN].astype(np.float32), (128, NOWN))),
        "kposc": np.ascontiguousarray(pos.reshape(32, 128).T.astype(np.float32)),
    })
    if kind == "fox":
        cpos = pos[::512] // 512
        Lc = (cpos[None, :] < cpos[:, None]).astype(np.float32)
        selh = np.zeros((24, 8, 128), np.float32)
        for h in range(8):
            selh[[h, 8 + h, 16 + h], h, :] = 1.0
        m.update({"w_in": np.ascontiguousarray(inputs["fox_w_in"][j]), "w_out": np.ascontiguousarray(inputs["fox_w_out"][j]),
                  "b_f": np.ascontiguousarray(inputs["fox_b_f"][j].reshape(8, 1)),
                  "Lc": np.ascontiguousarray(np.broadcast_to(Lc.reshape(1, 64), (8, 64))), "selh": selh})
    elif kind == "moba":
        esel = np.zeros((16, 16, 128), np.float32)
        for n in range(16):
            esel[n, n, :] = 1.0
        bpos = (pos[::256] // 256).astype(np.float32)
        m.update({"w_in": np.ascontiguousarray(inputs["moba_w_in"][j]), "w_out": np.ascontiguousarray(inputs["moba_w_out"][j]),
                  "esel": esel, "bposB": np.ascontiguousarray(np.broadcast_to(bpos[None], (128, 16))),
                  "ownb": np.ascontiguousarray((pos[:NOWN] // 256).reshape(16, 128).T.astype(np.float32))})
    else:
        inv_freq = np.exp(-np.log(10000.0) * np.arange(0, 256, 2, dtype=np.float32) / 256.0).astype(np.float32)
        ang = pos[None, :].astype(np.float32) * inv_freq[:, None]
        m.update({"w_in": np.ascontiguousarray(inputs["ret_w_in"][j]), "w_out": np.ascontiguousarray(inputs["ret_w_out"][j]),
                  "gngB": np.ascontiguousarray(np.broadcast_to(inputs["ret_gn_g"][j][None], (128, 2 * D))),
                  "cosT": np.cos(ang).astype(np.float32), "sinT": np.sin(ang).astype(np.float32)})
    return m


def own_index(z):
    return np.concatenate([np.arange(c * 512, (c + 1) * 512) for c in OWN_CHUNKS[z]])


_PROGS = {}


def get_prog(kind):
    if kind not in _PROGS:
        _PROGS[kind] = build_program(kind)
    return _PROGS[kind]


def run_moe_only(i, xmid, inputs):
    nc = get_prog("moe")
    common = moe_inputs(i, inputs)
    in_maps = []
    for c in range(8):
        b, z = c // 2, c % 2
        m = dict(common)
        m["xmid"] = np.ascontiguousarray(xmid[b][own_index(z)])
        in_maps.append(m)
    res = run_bass_kernel_spmd(nc, in_maps, core_ids=list(range(8)))
    out = np.zeros((NB, S, D), np.float32)
    for c in range(8):
        b, z = c // 2, c % 2
        out[b][own_index(z)] = res.results[c]["xout"]
    return out
```
